# Optimizing a Trainium2 kernel written in Bass

```python
import jax, jax.numpy as jnp
from jax import lax
import numpy as np

D_MODEL = 2048
BATCH = 1
SEQ = 8192
DEPTH = 1

POOL_WINDOWS = (2, 4, 8, 16)
N_POOL_GROUPS = len(POOL_WINDOWS)
POOL_WIDTH = D_MODEL // 2
POOL_GROUP = POOL_WIDTH // N_POOL_GROUPS
HEAD_DIM = 128
ATTN_WIDTH = D_MODEL // 2
ATTN_HEADS = ATTN_WIDTH // HEAD_DIM
MOBA_BLOCK = 256
MOBA_TOPK = 3
Q_CHUNK = 64
IN_PROJ_WIDTH = POOL_WIDTH + 3 * ATTN_WIDTH + 2 * D_MODEL
N_GROUPS = 4
EXPERTS_PER_GROUP = 4
N_EXPERTS = N_GROUPS * EXPERTS_PER_GROUP
EXPERT_TOPK = 2
D_EXPERT = D_MODEL // 2
RMS_EPS = 1e-6
NEG_INF = -1e30

kernel_name = "hybrid_pool_moba_hmoe_block"


def rms_norm(x, g):
    xf = x.astype(jnp.float32)
    y = xf * lax.rsqrt(jnp.mean(xf * xf, axis=-1, keepdims=True) + RMS_EPS)
    return (y * g.astype(jnp.float32)).astype(x.dtype)


def alibi_slopes(n_heads):
    return jnp.exp2(-8.0 * jnp.arange(1, n_heads + 1, dtype=jnp.float32) / n_heads)


def pool_mixer(u, w_pool, pool_scale):
    B, S, _ = u.shape
    ug = u.reshape(B, S, N_POOL_GROUPS, POOL_GROUP).astype(jnp.float32)
    cs = jnp.cumsum(ug, axis=1)
    t = jnp.arange(S)
    pooled = []
    for gi, w in enumerate(POOL_WINDOWS):
        c = cs[:, :, gi]
        c_prev = jnp.pad(c, ((0, 0), (w, 0), (0, 0)))[:, :S]
        cnt = jnp.minimum(t + 1, w).astype(jnp.float32)[None, :, None]
        pooled.append((c - c_prev) / cnt)
    pooled = jnp.stack(pooled, axis=2)
    mixed = (pooled - ug).astype(u.dtype)
    y = jnp.einsum('bsgc,gcd->bsgd', mixed, w_pool).reshape(B, S, POOL_WIDTH)
    return y * pool_scale


def moba_attention(q, k, v):
    B, S, H, Dh = q.shape
    s_pad = -(-S // MOBA_BLOCK) * MOBA_BLOCK
    pad = ((0, 0), (0, 0), (0, s_pad - S), (0, 0))
    qh = jnp.pad(q.transpose(0, 2, 1, 3), pad)
    kh = jnp.pad(k.transpose(0, 2, 1, 3), pad)
    vh = jnp.pad(v.transpose(0, 2, 1, 3), pad)
    nb = s_pad // MOBA_BLOCK
    topk = min(MOBA_TOPK, nb)
    kb = kh.reshape(B, H, nb, MOBA_BLOCK, Dh)
    vb = vh.reshape(B, H, nb, MOBA_BLOCK, Dh)
    kmean = jnp.mean(kb.astype(jnp.float32), axis=3)
    slopes = alibi_slopes(H)
    scale = HEAD_DIM ** -0.5
    nq = s_pad // Q_CHUNK
    qc = qh.reshape(B, H, nq, Q_CHUNK, Dh).transpose(2, 0, 1, 3, 4)
    b_ar = jnp.arange(B)[:, None, None, None]
    h_ar = jnp.arange(H)[None, :, None, None]
    blk_pos = jnp.arange(MOBA_BLOCK)

    def chunk(args):
        qi, ci = args
        t = ci * Q_CHUNK + jnp.arange(Q_CHUNK)
        own = (ci * Q_CHUNK) // MOBA_BLOCK
        gate = jnp.einsum('bhqd,bhnd->bhqn', qi.astype(jnp.float32), kmean)
        gate = jnp.where(jnp.arange(nb) < own, gate, NEG_INF)
        _, idx = lax.top_k(gate, topk)
        valid = idx < own
        kg = kb[b_ar, h_ar, idx]
        vg = vb[b_ar, h_ar, idx]
        s_sel = jnp.einsum('bhqd,bhqkcd->bhqkc', qi, kg).astype(jnp.float32) * scale
        pos_sel = idx[..., None] * MOBA_BLOCK + blk_pos
        dist_sel = (t[None, None, :, None, None] - pos_sel).astype(jnp.float32)
        s_sel = jnp.where(valid[..., None], s_sel - slopes[None, :, None, None, None] * dist_sel, NEG_INF)
        ko = lax.dynamic_index_in_dim(kb, own, axis=2, keepdims=False)
        vo = lax.dynamic_index_in_dim(vb, own, axis=2, keepdims=False)
        s_own = jnp.einsum('bhqd,bhcd->bhqc', qi, ko).astype(jnp.float32) * scale
        pos_own = own * MOBA_BLOCK + blk_pos
        dist_own = (t[:, None] - pos_own[None, :]).astype(jnp.float32)
        s_own = jnp.where((dist_own >= 0)[None, None], s_own - slopes[None, :, None, None] * dist_own[None, None], NEG_INF)
        scores = jnp.concatenate([s_sel.reshape(B, H, Q_CHUNK, topk * MOBA_BLOCK), s_own], axis=-1)
        p = jax.nn.softmax(scores, axis=-1).astype(v.dtype)
        p_sel = p[..., :topk * MOBA_BLOCK].reshape(B, H, Q_CHUNK, topk, MOBA_BLOCK)
        p_own = p[..., topk * MOBA_BLOCK:]
        return (jnp.einsum('bhqkc,bhqkcd->bhqd', p_sel, vg)
                + jnp.einsum('bhqc,bhcd->bhqd', p_own, vo))

    outs = lax.map(chunk, (qc, jnp.arange(nq, dtype=jnp.int32)))
    out = outs.transpose(1, 0, 3, 2, 4).reshape(B, s_pad, H * Dh)
    return out[:, :S]


def hier_moe(h, w_r_group, b_r_group, w_r_expert, b_r_expert, w_gate, w_up, w_down):
    hf = h.astype(jnp.float32)
    g_logits = hf @ w_r_group.astype(jnp.float32) + b_r_group.astype(jnp.float32)
    g_prob = jax.nn.softmax(g_logits, axis=-1)
    g_w, g_idx = lax.top_k(g_prob, 1)
    e_logits = jnp.einsum('bsd,gde->bsge', hf, w_r_expert.astype(jnp.float32)) + b_r_expert.astype(jnp.float32)
    e_logits = jnp.take_along_axis(e_logits, g_idx[..., None], axis=2)[:, :, 0]
    top_v, top_i = lax.top_k(e_logits, EXPERT_TOPK)
    top_w = jax.nn.softmax(top_v, axis=-1) * g_w
    flat_i = g_idx * EXPERTS_PER_GROUP + top_i
    combine = jnp.sum(jax.nn.one_hot(flat_i, N_EXPERTS, dtype=jnp.float32) * top_w[..., None], axis=-2)
    a = jnp.einsum('bsd,edf->bsef', h, w_gate)
    u = jnp.einsum('bsd,edf->bsef', h, w_up)
    hid = jax.nn.silu(a) * u * combine[..., None].astype(h.dtype)
    return jnp.einsum('bsef,efd->bsd', hid, w_down)


def setup_inputs(seed: int = 0) -> dict:
    key = jax.random.key(seed)
    ks = jax.random.split(key, 18)
    f32 = jnp.float32
    L = DEPTH

    def nrm(k, shape, scale):
        return jax.random.normal(k, shape, f32) * scale

    return {
        "x": nrm(ks[0], (BATCH, SEQ, D_MODEL), 1.0),
        "norm_mix": 1.0 + nrm(ks[1], (L, D_MODEL), 0.02),
        "w_in": nrm(ks[2], (L, D_MODEL, IN_PROJ_WIDTH), D_MODEL ** -0.5),
        "w_pool": nrm(ks[3], (L, N_POOL_GROUPS, POOL_GROUP, POOL_GROUP), POOL_GROUP ** -0.5),
        "pool_scale": 1.0 + nrm(ks[4], (L, POOL_WIDTH), 0.02),
        "w_branch_pool": nrm(ks[5], (L, POOL_WIDTH, D_MODEL), POOL_WIDTH ** -0.5),
        "w_branch_attn": nrm(ks[6], (L, ATTN_WIDTH, D_MODEL), ATTN_WIDTH ** -0.5),
        "w_out": nrm(ks[7], (L, D_MODEL, D_MODEL), D_MODEL ** -0.5),
        "norm_ffn": 1.0 + nrm(ks[8], (L, D_MODEL), 0.02),
        "w_r_group": nrm(ks[9], (L, D_MODEL, N_GROUPS), D_MODEL ** -0.5),
        "b_r_group": nrm(ks[10], (L, N_GROUPS), 0.01),
        "w_r_expert": nrm(ks[11], (L, N_GROUPS, D_MODEL, EXPERTS_PER_GROUP), D_MODEL ** -0.5),
        "b_r_expert": nrm(ks[12], (L, N_GROUPS, EXPERTS_PER_GROUP), 0.01),
        "w_gate": nrm(ks[13], (L, N_EXPERTS, D_MODEL, D_EXPERT), D_MODEL ** -0.5),
        "w_up": nrm(ks[14], (L, N_EXPERTS, D_MODEL, D_EXPERT), D_MODEL ** -0.5),
        "w_down": nrm(ks[15], (L, N_EXPERTS, D_EXPERT, D_MODEL), D_EXPERT ** -0.5),
        "norm_final": 1.0 + nrm(ks[16], (D_MODEL,), 0.02),
    }


def reference(x, norm_mix, w_in, w_pool, pool_scale, w_branch_pool, w_branch_attn, w_out,
              norm_ffn, w_r_group, b_r_group, w_r_expert, b_r_expert, w_gate, w_up, w_down,
              norm_final):
    B, S, _ = x.shape
    splits = [POOL_WIDTH, POOL_WIDTH + ATTN_WIDTH, POOL_WIDTH + 2 * ATTN_WIDTH,
              POOL_WIDTH + 3 * ATTN_WIDTH, POOL_WIDTH + 3 * ATTN_WIDTH + D_MODEL]
    for l in range(DEPTH):
        h = rms_norm(x, norm_mix[l])
        proj = h @ w_in[l]
        u_pool, q, k, v, gl_pool, gl_attn = jnp.split(proj, splits, axis=-1)
        y_pool = pool_mixer(u_pool, w_pool[l], pool_scale[l]) @ w_branch_pool[l]
        qh = q.reshape(B, S, ATTN_HEADS, HEAD_DIM)
        kh = k.reshape(B, S, ATTN_HEADS, HEAD_DIM)
        vh = v.reshape(B, S, ATTN_HEADS, HEAD_DIM)
        y_attn = moba_attention(qh, kh, vh) @ w_branch_attn[l]
        merged = jax.nn.sigmoid(gl_pool) * y_pool + jax.nn.sigmoid(gl_attn) * y_attn
        x = x + merged @ w_out[l]
        h = rms_norm(x, norm_ffn[l])
        x = x + hier_moe(h, w_r_group[l], b_r_group[l], w_r_expert[l], b_r_expert[l],
                         w_gate[l], w_up[l], w_down[l])
    return rms_norm(x, norm_final)
```

```python
import numpy as np
import concourse.bass as bass
import concourse.mybir as mybir
from concourse.bass_utils import run_bass_kernel_spmd

F32 = mybir.dt.float32
BF16 = mybir.dt.bfloat16
I32 = mybir.dt.int32
AF = mybir.ActivationFunctionType
ALU = mybir.AluOpType
AX = mybir.AxisListType

NEG = -1.0e30
SLOPES = [2.0 ** (-(h + 1)) for h in range(8)]
SCALE = 128.0 ** -0.5
EPS = 1e-6
NCORES = 8
KB = 1024


class Sched:
    def __init__(self, nc):
        self.nc = nc
        self.eng = {'pe': nc.tensor, 'act': nc.scalar, 'dve': nc.vector, 'pool': nc.gpsimd, 'sp': nc.sync}
        self.esem = {k: nc.alloc_semaphore(name=f"s_{k}") for k in ['pe', 'act', 'dve', 'pool']}
        self.ecnt = {k: 0 for k in self.esem}
        self.seen = {k: {} for k in self.eng}
        self.lastw = {}
        self.reads = {}
        self.dsem = {}
        self.dcnt = {}

    def _wait(self, e, tok):
        sem, val, src, sid = tok
        if src == e and e == 'pe':
            return
        if self.seen[e].get(sid, 0) >= val:
            return
        self.eng[e].wait_ge(sem, val)
        self.seen[e][sid] = val

    def _deps(self, e, reads, writes):
        for r in reads:
            if r in self.lastw:
                self._wait(e, self.lastw[r])
        for w in writes:
            if w in self.lastw:
                self._wait(e, self.lastw[w])
            for t in self.reads.get(w, {}).values():
                self._wait(e, t)

    def _commit(self, tok, reads, writes):
        for r in reads:
            self.reads.setdefault(r, {})[tok[3]] = tok
        for w in writes:
            self.lastw[w] = tok
            self.reads[w] = {}

    def op(self, e, fn, reads=(), writes=(), inc=True):
        self._deps(e, reads, writes)
        ins = fn(self.eng[e])
        if inc:
            self.ecnt[e] += 1
            ins.then_inc(self.esem[e], 1)
            tok = (self.esem[e], self.ecnt[e], e, 'E' + e)
        else:
            tok = (self.esem[e], self.ecnt[e] + 1, e, 'E' + e)
        self._commit(tok, reads, writes)
        return tok

    def dma(self, e, out, in_, semkey, reads=(), writes=()):
        if semkey not in self.dsem:
            self.dsem[semkey] = self.nc.alloc_semaphore(name=f"d_{semkey}")
            self.dcnt[semkey] = 0
        self._deps(e, reads, writes)
        ins = self.eng[e].dma_start(out=out, in_=in_)
        self.dcnt[semkey] += 16
        ins.then_inc(self.dsem[semkey], 16)
        tok = (self.dsem[semkey], self.dcnt[semkey], 'dma', 'D' + semkey)
        self._commit(tok, reads, writes)
        return tok

    def wait_tok(self, e, tok):
        self._wait(e, tok)

    def barrier(self):
        for e in self.eng:
            for e2, sem in self.esem.items():
                if self.ecnt[e2] > 0:
                    self._wait(e, (sem, self.ecnt[e2], e2, 'E' + e2))
            for k, sem in self.dsem.items():
                if self.dcnt[k] > 0:
                    self._wait(e, (sem, self.dcnt[k], 'dma', 'D' + k))


def v3(ap, a, b):
    return ap.rearrange("p (a b) -> p a b", a=a, b=b)


class Arena:
    def __init__(self, nc, nbytes):
        self.t = nc.alloc_sbuf_tensor("arena", [128, nbytes // 4], F32)
        self.nbytes = nbytes

    def f32(self, off, n):
        assert off % 4 == 0 and off + 4 * n <= self.nbytes, (off, n)
        return self.t[:, off // 4: off // 4 + n]

    def bf16(self, off, n):
        assert off % 4 == 0 and n % 2 == 0 and off + 2 * n <= self.nbytes, (off, n)
        return self.t[:, off // 4: off // 4 + n // 2].bitcast(BF16)


def build(stop_after=99, debug=False):
    nc = bass.Bass("TRN2", target_bir_lowering=False)

    def din(name, shape, dtype=F32):
        return nc.dram_tensor(name, list(shape), dtype, kind="ExternalInput").ap()

    xc = din("xc", [8192, 2048])
    w_in = din("w_in", [2048, 8192])
    w_pool = din("w_pool", [4, 256, 256])
    w_bp = din("w_bp", [1024, 2048])
    w_ba = din("w_ba", [1024, 2048])
    w_out = din("w_out", [2048, 2048])
    w_gate = din("w_gate", [16, 2048, 1024])
    w_up = din("w_up", [16, 2048, 1024])
    w_down = din("w_down", [16, 1024, 2048])
    g1_d = din("g1", [128, 2048])
    g2_d = din("g2", [128, 2048])
    g3_d = din("g3", [128, 2048])
    pst_d = din("pst", [128, 8])
    wr_d = din("wr", [128, 16 * 20])
    br_d = din("br", [128, 20])
    padmask_d = din("padmask", [128, 32])
    invcnt_d = din("invcnt", [128, 8 * 16])
    out_d = nc.dram_tensor("out", [1024, 2048], F32, kind="ExternalOutput").ap()
    kt_d = nc.dram_tensor("kt_scr", [8, 128, 8192], BF16, kind="Internal").ap()
    vs_d = nc.dram_tensor("vs_scr", [8192, 1024], BF16, kind="Internal").ap()

    dbg_outs = []

    S = Sched(nc)
    A = Arena(nc, 206 * KB)
    banks = [nc.alloc_psum_tensor(f"bank{i}", [128, 512], F32) for i in range(8)]
    B = [b[:, :] for b in banks]
    Bb = [b[:, :].bitcast(BF16) for b in banks]

    def dbg(name, ap, shape, dtype, reads):
        if not debug:
            return
        d = nc.dram_tensor("dbg_" + name, list(shape), dtype, kind="ExternalOutput").ap()
        dbg_outs.append(S.dma('sp', d, ap, 'dbg_' + name, reads=reads))

    def finish(extra=()):
        for t in list(extra) + dbg_outs:
            S.wait_tok('sp', t)
        return nc

    o = 0

    def cf32(n):
        nonlocal o
        ap = A.f32(o, n)
        o += 4 * n
        return ap

    def cbf(n):
        nonlocal o
        ap = A.bf16(o, n)
        o += 2 * n
        return ap

    ident_b = cbf(128)
    tri = cbf(128)
    ones_b = cbf(128)
    ident_f = cf32(128)
    ones_f = cf32(128)
    ksum = v3(cf32(256), 8, 32)
    padmask = cf32(32)
    invcnt = v3(cf32(128), 8, 16)
    pst = cf32(8)
    kbias = cf32(8)
    iop = cf32(1)
    Dall = v3(cf32(224), 8, 28)
    Dnr = cf32(8)
    NWr = [cf32(8) for _ in range(2)]
    ssq = cf32(64)
    rstd = cf32(64)
    m8 = cf32(8)
    selt = cf32(32)
    rden = cf32(2)
    br = cf32(20)
    epst = cf32(1)
    itmp = A.t[:, o // 4: o // 4 + 224].bitcast(I32)
    o += 4 * 224
    assert o <= 8 * KB, o

    S.op('pool', lambda e: e.memset(ones_b, 1.0), writes=['ones_b'])
    S.op('pool', lambda e: e.memset(ones_f, 1.0), writes=['ones_f'])
    S.op('pool', lambda e: e.affine_select(out=ident_b, in_=ones_b, pattern=[[-1, 128]], compare_op=ALU.is_equal,
                                           fill=0.0, base=0, channel_multiplier=1), reads=['ones_b'], writes=['ident_b'])
    S.op('pool', lambda e: e.affine_select(out=ident_f, in_=ones_f, pattern=[[-1, 128]], compare_op=ALU.is_equal,
                                           fill=0.0, base=0, channel_multiplier=1), reads=['ones_f'], writes=['ident_f'])
    S.op('pool', lambda e: e.affine_select(out=tri, in_=ones_b, pattern=[[1, 128]], compare_op=ALU.is_ge,
                                           fill=0.0, base=0, channel_multiplier=-1), reads=['ones_b'], writes=['tri'])
    S.op('pool', lambda e: e.iota(v3(itmp[:, 0:224], 8, 28), pattern=[[128, 8], [-256, 28]], base=6913, channel_multiplier=1),
         writes=['itmp'])
    S.op('dve', lambda e: e.tensor_copy(out=Dall, in_=v3(itmp[:, 0:224], 8, 28)), reads=['itmp'], writes=['Dall'])
    S.op('pool', lambda e: e.iota(itmp[:, 0:8], pattern=[[-128, 8]], base=769, channel_multiplier=1),
         reads=['Dall'], writes=['itmp'])
    S.op('dve', lambda e: e.tensor_copy(out=Dnr, in_=itmp[:, 0:8]), reads=['itmp'], writes=['Dnr'])
    S.op('pool', lambda e: e.iota(itmp[:, 0:1], pattern=[[0, 1]], base=-127, channel_multiplier=1),
         reads=['Dnr'], writes=['itmp'])
    S.op('dve', lambda e: e.tensor_copy(out=iop, in_=itmp[:, 0:1]), reads=['itmp'], writes=['iop'])
    for h in range(8):
        S.op('dve', lambda e: e.tensor_scalar(out=kbias[:, h:h + 1], in0=iop, scalar1=SLOPES[h], scalar2=None,
                                              op0=ALU.mult), reads=['iop'], writes=['kbias'])
    S.op('dve', lambda e: e.memset(ksum, 0.0), writes=['ksum'])
    S.op('dve', lambda e: e.memset(epst, EPS), writes=['epst'])
    S.op('dve', lambda e: e.memset(ssq, 0.0), writes=['ssq'])
    S.dma('sp', padmask, padmask_d, 'c0', writes=['padmask'])
    S.dma('sp', invcnt, v3(invcnt_d, 8, 16), 'c1', writes=['invcnt'])
    S.dma('sp', pst, pst_d, 'c2', writes=['pst'])
    S.dma('sp', br, br_d, 'c3', writes=['br'])

    hT = [v3(A.bf16(8 * KB + i * 16 * KB, 8192), 16, 512) for i in range(2)]
    hTh = v3(A.bf16(40 * KB, 2048), 16, 128)
    W = [v3(A.bf16(44 * KB + i * 16 * KB, 8192), 16, 512) for i in range(4)]
    g1bc = A.f32(108 * KB, 2048)
    xs = [A.f32(116 * KB + i * 8 * KB, 2048) for i in range(3)]
    xb = [A.bf16(140 * KB + i * 4 * KB, 2048) for i in range(2)]
    kst = [v3(A.bf16(148 * KB + i * 8 * KB, 4096), 8, 512) for i in range(2)]
    vst = [v3(A.bf16(164 * KB + i * 8 * KB, 4096), 4, 1024) for i in range(2)]
    junk = A.bf16(180 * KB, 2048)

    def load_w_in(q, c0, key):
        return S.dma('pool', W[q], w_in[:, c0:c0 + 512].rearrange("(c p) f -> p c f", p=128), key, writes=[key])

    S.dma('sp', g1bc, g1_d, 'g1bc', writes=['g1bc'])
    for q in range(4):
        load_w_in(q, 2048 + q * 512, f'W{q}')

    store_toks = []
    for g in range(16):
        hs = g % 2
        for t in range(4):
            i = 4 * g + t
            xi, bi = i % 3, i % 2
            S.dma('sp', xs[xi], xc[i * 128:(i + 1) * 128, :], f'xs{xi}', writes=[f'xs{xi}'])
            S.op('act', lambda e: e.activation(out=junk, in_=xs[xi], func=AF.Square, accum_out=ssq[:, i:i + 1]),
                 reads=[f'xs{xi}', 'ssq'], writes=['junk', f'ssq{i}'])
            S.op('act', lambda e: e.activation(out=rstd[:, i:i + 1], in_=ssq[:, i:i + 1], func=AF.Sqrt,
                                               scale=1.0 / 2048, bias=epst), reads=[f'ssq{i}', 'epst'], writes=[f'rstd{i}'])
            S.op('dve', lambda e: e.reciprocal(out=rstd[:, i:i + 1], in_=rstd[:, i:i + 1]),
                 reads=[f'rstd{i}'], writes=[f'rstd{i}'])
            S.op('dve', lambda e: e.scalar_tensor_tensor(out=xb[bi], in0=xs[xi], scalar=rstd[:, i:i + 1], in1=g1bc,
                                                         op0=ALU.mult, op1=ALU.mult),
                 reads=[f'xs{xi}', f'rstd{i}', 'g1bc'], writes=[f'xb{bi}'])
            for half in range(2):
                bk = (i % 2) * 2 + half
                for k in range(8):
                    dc = half * 8 + k
                    S.op('pe', lambda e: e.transpose(Bb[bk][:, k * 128:(k + 1) * 128], xb[bi][:, dc * 128:(dc + 1) * 128],
                                                     ident_b),
                         reads=[f'xb{bi}', 'ident_b'], writes=[f'B{bk}'], inc=(k == 7))
                S.op('act', lambda e: e.activation(out=hT[hs][:, half * 8:(half + 1) * 8, t * 128:(t + 1) * 128],
                                                   in_=v3(Bb[bk], 8, 128), func=AF.Copy),
                     reads=[f'B{bk}'], writes=[f'hT{hs}_{t}_{half}'])
        hkeys = [f'hT{hs}_{t}_{half}' for t in range(4) for half in range(2)]
        if g == 13:
            S.op('dve', lambda e: e.tensor_copy(out=hTh, in_=hT[hs][:, :, 384:512]), reads=hkeys, writes=['hTh'])
        ks = g % 2
        for h in range(8):
            pk = 4 + h % 2
            for dc in range(16):
                S.op('pe', lambda e: e.matmul(B[pk], lhsT=W[h // 4][:, dc, (h % 4) * 128:(h % 4 + 1) * 128],
                                              rhs=hT[hs][:, dc, :], start=(dc == 0), stop=(dc == 15)),
                     reads=[f'W{h // 4}'] + hkeys, writes=[f'B{pk}'], inc=(dc == 15))
            for half in range(2):
                S.op('act', lambda e: e.activation(out=kst[ks][:, h, half * 256:(half + 1) * 256],
                                                   in_=B[pk][:, half * 256:(half + 1) * 256], func=AF.Copy,
                                                   accum_out=ksum[:, h, 2 * g + half:2 * g + half + 1]),
                     reads=[f'B{pk}', 'ksum'], writes=[f'kst{ks}', f'ksum{g}'])
        store_toks.append(S.dma('pool', kt_d[:, :, g * 512:(g + 1) * 512].rearrange("h p t -> p h t"), kst[ks],
                                f'kstS{ks}', reads=[f'kst{ks}']))
        vsl = g % 2
        for t in range(4):
            for cb in range(2):
                pv = 6 + (t * 2 + cb) % 2
                for dc in range(16):
                    S.op('pe', lambda e: e.matmul(B[pv], lhsT=hT[hs][:, dc, t * 128:(t + 1) * 128],
                                                  rhs=W[2 + cb][:, dc, :], start=(dc == 0), stop=(dc == 15)),
                         reads=[f'W{2 + cb}'] + hkeys, writes=[f'B{pv}'], inc=(dc == 15))
                S.op('dve', lambda e: e.tensor_copy(out=vst[vsl][:, t, cb * 512:(cb + 1) * 512], in_=B[pv]),
                     reads=[f'B{pv}'], writes=[f'vst{vsl}'])
        store_toks.append(S.dma('pool', vs_d[g * 512:(g + 1) * 512, :].rearrange("(t p) d -> p t d", p=128), vst[vsl],
                                f'vstS{vsl}', reads=[f'vst{vsl}']))
    hk = [[f'hT{hs}_{t}_{half}' for t in range(4) for half in range(2)] for hs in range(2)]
    dbg("ksum", ksum, [128, 8, 32], F32, ['ksum'] + [f'ksum{g}' for g in range(16)])
    dbg("rstd", rstd, [128, 64], F32, [f'rstd{i}' for i in range(64)])
    dbg("hT0", hT[0], [128, 16, 512], BF16, hk[0])
    if stop_after <= 1:
        return finish(store_toks)

    uT = v3(A.f32(108 * KB, 8 * 1040), 8, 1040)
    tA = v3(A.f32(141 * KB, 2080), 2, 1040)
    tB = v3(A.f32(150 * KB, 2080), 2, 1040)
    mixT = v3(A.bf16(159 * KB, 8192), 8, 1024)
    wpool = A.bf16(175 * KB, 2048).rearrange("p (g i o) -> p g i o", g=4, i=2, o=256)
    qhi = [A.bf16(179 * KB + i * KB, 512) for i in range(2)]
    qlo = [A.bf16(181 * KB + i * KB, 512) for i in range(2)]
    kshi = v3(A.bf16(192 * KB, 256), 8, 32)
    kslo = v3(A.bf16(192 * KB + 512, 256), 8, 32)
    gate = A.f32(183 * KB, 2048).rearrange("p (h q n) -> p h q n", h=8, q=8, n=32)
    mtmp = v3(A.f32(191 * KB, 32), 2, 16)
    zT = v3(A.bf16(44 * KB, 8192), 8, 1024)
    QT = v3(A.bf16(60 * KB, 8192), 8, 1024)

    S.barrier()
    load_w_in(0, 0, 'W0')
    load_w_in(1, 512, 'W1')
    load_w_in(2, 1024, 'W2')
    load_w_in(3, 1536, 'W3')
    S.dma('pool', wpool, w_pool.rearrange("g (i p) o -> p g i o", p=128), 'wpool', writes=['wpool'])

    pi = 0
    for c in range(8):
        for th in range(2):
            pb = pi % 4
            pi += 1
            for dc in range(16):
                S.op('pe', lambda e: e.matmul(B[pb], lhsT=W[c // 4][:, dc, (c % 4) * 128:(c % 4 + 1) * 128],
                                              rhs=hT[th][:, dc, :], start=(dc == 0), stop=(dc == 15)),
                     reads=[f'W{c // 4}'] + hk[th], writes=[f'B{pb}'], inc=(dc == 15))
            S.op('act', lambda e: e.activation(out=uT[:, c, 16 + th * 512:16 + (th + 1) * 512], in_=B[pb], func=AF.Copy),
                 reads=[f'B{pb}'], writes=[f'uT{c}'])
        pb = pi % 4
        pi += 1
        for dc in range(16):
            S.op('pe', lambda e: e.matmul(B[pb][:, 0:16], lhsT=W[c // 4][:, dc, (c % 4) * 128:(c % 4 + 1) * 128],
                                          rhs=hTh[:, dc, 112:128], start=(dc == 0), stop=(dc == 15)),
                 reads=[f'W{c // 4}', 'hTh'], writes=[f'B{pb}'], inc=(dc == 15))
        S.op('act', lambda e: e.activation(out=uT[:, c, 0:16], in_=B[pb][:, 0:16], func=AF.Copy),
             reads=[f'B{pb}'], writes=[f'uT{c}'])
    dbg("uT", uT, [128, 8, 1040], F32, [f'uT{c}' for c in range(8)])
    for gi in range(4):
        src = uT[:, 2 * gi:2 * gi + 2, :]
        cur, ckey = src, None
        ukeys = [f'uT{2 * gi}', f'uT{2 * gi + 1}']
        for k in range(gi + 1):
            sh, lo = 2 ** k, 2 ** (k + 1) - 1
            dst, dkey = (tA, 'tA') if k % 2 == 0 else (tB, 'tB')
            S.op('dve', lambda e: e.tensor_tensor(out=dst[:, :, lo:1040], in0=cur[:, :, lo:1040],
                                                  in1=cur[:, :, lo - sh:1040 - sh], op=ALU.add),
                 reads=ukeys + ([ckey] if ckey else []), writes=[dkey])
            cur, ckey = dst, dkey
        w = 2 ** (gi + 1)
        S.op('dve', lambda e: e.scalar_tensor_tensor(out=mixT[:, 2 * gi:2 * gi + 2, :], in0=cur[:, :, 16:1040],
                                                     scalar=1.0 / w, in1=src[:, :, 16:1040], op0=ALU.mult,
                                                     op1=ALU.subtract),
             reads=ukeys + [ckey], writes=[f'mix{gi}'])
        S.op('dve', lambda e: e.tensor_tensor(out=mtmp, in0=cur[:, :, 16:32], in1=invcnt[:, 2 * gi:2 * gi + 2, :],
                                              op=ALU.mult), reads=[ckey, 'invcnt'], writes=['mtmp'])
        S.op('dve', lambda e: e.tensor_tensor(out=mixT[:, 2 * gi:2 * gi + 2, 0:16], in0=mtmp, in1=src[:, :, 16:32],
                                              op=ALU.subtract), reads=['mtmp', f'mix{gi}'] + ukeys, writes=[f'mix{gi}'])
    dbg("mixT", mixT, [128, 8, 1024], BF16, [f'mix{gi}' for gi in range(4)])
    for gi in range(4):
        for oc in range(2):
            for th in range(2):
                pb = pi % 4
                pi += 1
                for ic in range(2):
                    S.op('pe', lambda e: e.matmul(B[pb], lhsT=wpool[:, gi, ic, oc * 128:(oc + 1) * 128],
                                                  rhs=mixT[:, 2 * gi + ic, th * 512:(th + 1) * 512],
                                                  start=(ic == 0), stop=(ic == 1)),
                         reads=['wpool', f'mix{gi}'], writes=[f'B{pb}'], inc=(ic == 1))
                S.op('dve', lambda e: e.tensor_scalar(out=zT[:, 2 * gi + oc, th * 512:(th + 1) * 512], in0=B[pb],
                                                      scalar1=pst[:, 2 * gi + oc:2 * gi + oc + 1], scalar2=None,
                                                      op0=ALU.mult),
                     reads=[f'B{pb}', 'pst'], writes=['zT', 'W0'])
    dbg("zT", zT, [128, 8, 1024], BF16, ['zT'])
    if stop_after <= 1.5:
        return finish(store_toks)

    kall = ['ksum'] + [f'ksum{g}' for g in range(16)]
    S.op('dve', lambda e: e.tensor_copy(out=kshi, in_=ksum), reads=kall, writes=['kshi'])
    S.op('dve', lambda e: e.scalar_tensor_tensor(out=kslo, in0=kshi, scalar=-1.0, in1=ksum, op0=ALU.mult, op1=ALU.add), reads=kall + ['kshi'],
         writes=['kslo'])
    for h in range(8):
        for th in range(2):
            pb = pi % 4
            pi += 1
            qs = (h * 2 + th) % 2
            for dc in range(16):
                S.op('pe', lambda e: e.matmul(B[pb], lhsT=W[2 + h // 4][:, dc, (h % 4) * 128:(h % 4 + 1) * 128],
                                              rhs=hT[th][:, dc, :], start=(dc == 0), stop=(dc == 15)),
                     reads=[f'W{2 + h // 4}'] + hk[th], writes=[f'B{pb}'], inc=(dc == 15))
            S.op('act', lambda e: e.activation(out=QT[:, h, th * 512:(th + 1) * 512], in_=B[pb], func=AF.Copy,
                                               scale=SCALE), reads=[f'B{pb}'], writes=['QT', 'W1', f'Bsync{pb}'])
            if stop_after <= 1.6:
                continue
            S.op('dve', lambda e: e.tensor_copy(out=qhi[qs], in_=B[pb]), reads=[f'B{pb}', f'Bsync{pb}'], writes=[f'qhi{qs}'])
            S.op('dve', lambda e: e.scalar_tensor_tensor(out=qlo[qs], in0=qhi[qs], scalar=-1.0, in1=B[pb], op0=ALU.mult, op1=ALU.add),
                 reads=[f'B{pb}', f'qhi{qs}'], writes=[f'qlo{qs}'])
            if stop_after <= 1.65:
                continue
            for qi in range(4):
                qt = th * 4 + qi
                pg = 4 + qt % 4
                qsl_ = slice(qi * 128, (qi + 1) * 128)
                S.op('pe', lambda e: e.matmul(B[pg][:, 0:32], lhsT=qhi[qs][:, qsl_], rhs=kshi[:, h, :], start=True,
                                              stop=False), reads=[f'qhi{qs}', f'qlo{qs}', 'kshi', 'kslo'],
                     writes=[f'B{pg}'], inc=False)
                S.op('pe', lambda e: e.matmul(B[pg][:, 0:32], lhsT=qhi[qs][:, qsl_], rhs=kslo[:, h, :], start=False,
                                              stop=False), reads=[f'qhi{qs}', f'qlo{qs}', 'kshi', 'kslo'],
                     writes=[f'B{pg}'], inc=False)
                S.op('pe', lambda e: e.matmul(B[pg][:, 0:32], lhsT=qlo[qs][:, qsl_], rhs=kshi[:, h, :], start=False,
                                              stop=True), reads=[f'qhi{qs}', f'qlo{qs}', 'kshi', 'kslo'],
                     writes=[f'B{pg}'])
                S.op('dve', lambda e: e.tensor_tensor(out=gate[:, h, qt, :], in0=B[pg][:, 0:32], in1=padmask,
                                                      op=ALU.add),
                     reads=[f'B{pg}', 'padmask'], writes=['gate'])
    if stop_after > 1.65:
        dbg("gate", gate, [128, 8, 8, 32], F32, ['gate'])
    dbg("QT", QT, [128, 8, 1024], BF16, ['QT'])
    if stop_after <= 1.7:
        return finish(store_toks)
    for h in range(8):
        for qt in range(8):
            j = qt // 2
            gv = gate[:, h, qt, :]
            if 28 + j < 32:
                S.op('dve', lambda e: e.memset(gate[:, h, qt, 28 + j:32], NEG), reads=['gate'], writes=['gate'])
            S.op('dve', lambda e: e.max(out=m8, in_=gv), reads=['gate'], writes=['m8'])
            S.op('dve', lambda e: e.tensor_scalar(out=selt, in0=gv, scalar1=m8[:, 2:3], scalar2=None, op0=ALU.is_ge),
                 reads=['gate', 'm8'], writes=['selt'])
            S.op('dve', lambda e: e.scalar_tensor_tensor(out=gv, in0=gv, scalar=-1.0e29, in1=selt, op0=ALU.is_gt,
                                                         op1=ALU.mult),
                 reads=['gate', 'selt'], writes=['gate'])
    sel = gate
    dbg("sel", sel, [128, 8, 8, 32], F32, ['gate'])
    if stop_after <= 2:
        return finish(store_toks)

    KTb = [A.bf16(76 * KB + i * 16 * KB, 8192) for i in range(2)]
    Vh = [v3(A.bf16(108 * KB + i * 16640, 64 * 129 + 64), 64, 130)[:, :, 0:129] for i in range(2)]
    PT = [A.bf16(142 * KB + i * KB, 512) for i in range(3)]
    Oacc = [v3(A.f32(145 * KB + i * 1280, 258), 2, 129) for i in range(2)]
    atok = [A.bf16(148 * KB + i * 256, 128) for i in range(2)]
    attnT = v3(A.bf16(149 * KB, 8192), 8, 1024)
    Ef = [v3(A.f32(165 * KB + i * KB, 224), 8, 28) for i in range(2)]
    Wn = [v3(A.f32(167 * KB + i * 256, 64), 8, 8) for i in range(2)]
    S.barrier()
    for t in store_toks:
        S.wait_tok('sp', t)
    si = oi = pti = 0
    for h in range(8):
        hb = h % 2
        sl = SLOPES[h]
        S.dma('sp', KTb[hb], kt_d[h], f'KT{hb}', writes=[f'KT{hb}'])
        for c0 in range(0, 64, 16):
            S.dma('sp', Vh[hb][:, c0:c0 + 16, 0:128],
                  vs_d[c0 * 128:(c0 + 16) * 128, h * 128:(h + 1) * 128].rearrange("(c p) d -> p c d", p=128),
                  f'V{hb}', writes=[f'V{hb}'])
        S.op('dve', lambda e: e.memset(Vh[hb][:, :, 128:129], 1.0), reads=[f'V{hb}'], writes=[f'V{hb}'])
        vev = Vh[hb][:, 0:56, :].rearrange("p (b two) d -> p b two d", two=2)[:, :, 0, :]
        S.op('dve', lambda e: e.tensor_scalar(out=vev, in0=vev, scalar1=float(np.exp(-sl * 128.0)), scalar2=None,
                                              op0=ALU.mult), reads=[f'V{hb}'], writes=[f'V{hb}'])
        S.op('act', lambda e: e.activation(out=Ef[hb], in_=Dall, func=AF.Exp, scale=-sl),
             reads=['Dall'], writes=[f'Ef{hb}'])
        S.op('dve', lambda e: e.tensor_tensor(out=Ef[hb][:, :, :], in0=Ef[hb][:, :, :], in1=sel[:, h, :, 0:28],
                                              op=ALU.mult), reads=[f'Ef{hb}', 'gate'], writes=[f'Ef{hb}'])
        S.op('act', lambda e: e.activation(out=NWr[hb], in_=Dnr, func=AF.Exp, scale=-sl),
             reads=['Dnr'], writes=[f'NWr{hb}'])
        for qt in range(8):
            for b in range(qt // 2):
                S.op('dve', lambda e: e.tensor_scalar(out=Wn[hb][:, qt, 2 * b:2 * b + 2],
                                                      in0=NWr[hb][:, 7 - qt + 2 * b:7 - qt + 2 * b + 2],
                                                      scalar1=sel[:, h, qt, 28 + b:29 + b], scalar2=None, op0=ALU.mult),
                     reads=[f'NWr{hb}', 'gate'], writes=[f'Wn{hb}'])
        for j in range(4):
            ob = (h * 4 + j) % 2
            qsl = QT[:, h, j * 256:(j + 1) * 256]
            S.op('dve', lambda e: e.memset(Oacc[ob], 0.0), writes=[f'Oacc{ob}'])
            items = [('far', n) for n in range(28)] + [('near', c) for c in range(2 * j + 2)]

            def stage1(it):
                nonlocal si, pti
                kind, idx = it
                ps = si % 3
                si += 1
                pt = pti % 3
                pti += 1
                if kind == 'far':
                    n = idx
                    for kc in range(2):
                        S.op('pe', lambda e: e.matmul(B[ps][:, kc * 256:(kc + 1) * 256],
                                                      lhsT=KTb[hb][:, n * 256 + kc * 128:n * 256 + (kc + 1) * 128],
                                                      rhs=qsl, start=True, stop=True),
                             reads=[f'KT{hb}', 'QT'], writes=[f'B{ps}'], inc=(kc == 1))
                    S.op('act', lambda e: e.activation(out=PT[pt], in_=B[ps], func=AF.Exp, bias=kbias[:, h:h + 1]),
                         reads=[f'B{ps}', 'kbias'], writes=[f'PT{pt}'])
                else:
                    c = idx
                    S.op('pe', lambda e: e.matmul(B[ps][:, 0:256], lhsT=KTb[hb][:, (56 + c) * 128:(57 + c) * 128],
                                                  rhs=qsl, start=True, stop=True),
                         reads=[f'KT{hb}', 'QT'], writes=[f'B{ps}'])
                    S.op('act', lambda e: e.activation(out=PT[pt][:, 0:256], in_=B[ps][:, 0:256], func=AF.Exp,
                                                       bias=kbias[:, h:h + 1]),
                         reads=[f'B{ps}', 'kbias'], writes=[f'PT{pt}'])
                return pt

            def stage2(it, pt):
                nonlocal oi
                kind, idx = it
                if kind == 'far':
                    n = idx
                    for qc in range(2):
                        po = 3 + oi % 4
                        oi += 1
                        for kc in range(2):
                            S.op('pe', lambda e: e.matmul(B[po][:, 0:129],
                                                          lhsT=PT[pt][:, kc * 256 + qc * 128:kc * 256 + (qc + 1) * 128],
                                                          rhs=Vh[hb][:, 2 * n + kc, :], start=(kc == 0), stop=(kc == 1)),
                                 reads=[f'PT{pt}', f'V{hb}'], writes=[f'B{po}'], inc=(kc == 1))
                        S.op('dve', lambda e: e.scalar_tensor_tensor(out=Oacc[ob][:, qc, :], in0=B[po][:, 0:129],
                                                                     scalar=Ef[hb][:, 2 * j + qc, n:n + 1],
                                                                     in1=Oacc[ob][:, qc, :], op0=ALU.mult, op1=ALU.add),
                             reads=[f'B{po}', f'Ef{hb}', f'Oacc{ob}'], writes=[f'Oacc{ob}'])
                else:
                    c = idx
                    for qc in range(2):
                        qt = 2 * j + qc
                        if c > qt:
                            continue
                        if c == qt:
                            S.op('dve', lambda e: e.tensor_tensor(out=PT[pt][:, qc * 128:(qc + 1) * 128],
                                                                  in0=PT[pt][:, qc * 128:(qc + 1) * 128], in1=tri,
                                                                  op=ALU.mult),
                                 reads=[f'PT{pt}', 'tri'], writes=[f'PT{pt}'])
                        po = 3 + oi % 4
                        oi += 1
                        S.op('pe', lambda e: e.matmul(B[po][:, 0:129], lhsT=PT[pt][:, qc * 128:(qc + 1) * 128],
                                                      rhs=Vh[hb][:, 56 + c, :], start=True, stop=True),
                             reads=[f'PT{pt}', f'V{hb}'], writes=[f'B{po}'])
                        if c // 2 < qt // 2:
                            wap, wkey = Wn[hb][:, qt, c:c + 1], f'Wn{hb}'
                        else:
                            wap, wkey = NWr[hb][:, 7 - qt + c:8 - qt + c], f'NWr{hb}'
                        S.op('dve', lambda e: e.scalar_tensor_tensor(out=Oacc[ob][:, qc, :], in0=B[po][:, 0:129],
                                                                     scalar=wap, in1=Oacc[ob][:, qc, :], op0=ALU.mult,
                                                                     op1=ALU.add),
                             reads=[f'B{po}', wkey, f'Oacc{ob}'], writes=[f'Oacc{ob}'])

            prev = None
            for it in items:
                ptc = stage1(it)
                if prev is not None:
                    stage2(*prev)
                prev = (it, ptc)
            stage2(*prev)
            for qc in range(2):
                qt = 2 * j + qc
                S.op('dve', lambda e: e.reciprocal(out=rden[:, qc:qc + 1], in_=Oacc[ob][:, qc, 128:129]),
                     reads=[f'Oacc{ob}'], writes=[f'rden{qc}'])
                S.op('dve', lambda e: e.tensor_scalar(out=atok[qc], in0=Oacc[ob][:, qc, 0:128],
                                                      scalar1=rden[:, qc:qc + 1], scalar2=None, op0=ALU.mult),
                     reads=[f'Oacc{ob}', f'rden{qc}'], writes=[f'atok{qc}'])
                S.op('pe', lambda e: e.transpose(Bb[7][:, qc * 128:(qc + 1) * 128], atok[qc], ident_b),
                     reads=[f'atok{qc}', 'ident_b'], writes=['B7'])
                S.op('act', lambda e: e.activation(out=attnT[:, h, qt * 128:(qt + 1) * 128],
                                                   in_=Bb[7][:, qc * 128:(qc + 1) * 128], func=AF.Copy),
                     reads=['B7'], writes=['attnT'])
    dbg("attnT", attnT, [128, 8, 1024], BF16, ['attnT'])
    if stop_after <= 3:
        return finish()

    mergedT = v3(A.bf16(76 * KB, 16384), 16, 1024)
    U4 = [108 * KB, 165 * KB]
    wgp = [v3(A.bf16(U4[i], 4096), 16, 256) for i in range(2)]
    wga = [v3(A.bf16(U4[i] + 8 * KB, 4096), 16, 256) for i in range(2)]
    wbp = [v3(A.bf16(U4[i] + 16 * KB, 2048), 8, 256) for i in range(2)]
    wba = [v3(A.bf16(U4[i] + 20 * KB, 2048), 8, 256) for i in range(2)]
    sg1 = [A.f32(132 * KB + i * 2 * KB, 512) for i in range(2)]
    sg2 = [A.f32(136 * KB + i * 2 * KB, 512) for i in range(2)]
    S.barrier()

    def load_u4(u):
        s = u % 2
        extra = []
        S.dma('pool', wgp[s], w_in[:, 4096 + u * 256:4096 + (u + 1) * 256].rearrange("(c p) f -> p c f", p=128),
              f'wgp{s}', writes=[f'wgp{s}'] + extra)
        S.dma('pool', wga[s], w_in[:, 6144 + u * 256:6144 + (u + 1) * 256].rearrange("(c p) f -> p c f", p=128),
              f'wga{s}', writes=[f'wga{s}'] + extra)
        S.dma('pool', wbp[s], w_bp[:, u * 256:(u + 1) * 256].rearrange("(c p) f -> p c f", p=128),
              f'wbp{s}', writes=[f'wbp{s}'] + extra)
        S.dma('pool', wba[s], w_ba[:, u * 256:(u + 1) * 256].rearrange("(c p) f -> p c f", p=128),
              f'wba{s}', writes=[f'wba{s}'] + extra)

    load_u4(0)
    load_u4(1)
    ui = 0
    for u in range(8):
        s = u % 2
        for fl in range(2):
            fcx = u * 2 + fl
            fs = slice(fl * 128, (fl + 1) * 128)
            for th in range(2):
                par = ui % 2
                ui += 1
                ts_ = slice(th * 512, (th + 1) * 512)
                for dc in range(16):
                    S.op('pe', lambda e: e.matmul(B[0 + par], lhsT=wgp[s][:, dc, fs], rhs=hT[th][:, dc, :],
                                                  start=(dc == 0), stop=(dc == 15)),
                         reads=[f'wgp{s}'] + hk[th], writes=[f'B{par}'], inc=(dc == 15))
                for dc in range(16):
                    S.op('pe', lambda e: e.matmul(B[2 + par], lhsT=wga[s][:, dc, fs], rhs=hT[th][:, dc, :],
                                                  start=(dc == 0), stop=(dc == 15)),
                         reads=[f'wga{s}'] + hk[th], writes=[f'B{2 + par}'], inc=(dc == 15))
                for c in range(8):
                    S.op('pe', lambda e: e.matmul(B[4 + par], lhsT=wbp[s][:, c, fs], rhs=zT[:, c, ts_],
                                                  start=(c == 0), stop=(c == 7)),
                         reads=[f'wbp{s}', 'zT'], writes=[f'B{4 + par}'], inc=(c == 7))
                for c in range(8):
                    S.op('pe', lambda e: e.matmul(B[6 + par], lhsT=wba[s][:, c, fs], rhs=attnT[:, c, ts_],
                                                  start=(c == 0), stop=(c == 7)),
                         reads=[f'wba{s}', 'attnT'], writes=[f'B{6 + par}'], inc=(c == 7))
                S.op('act', lambda e: e.activation(out=sg1[par], in_=B[0 + par], func=AF.Sigmoid),
                     reads=[f'B{par}'], writes=[f'sg1{par}'])
                S.op('act', lambda e: e.activation(out=sg2[par], in_=B[2 + par], func=AF.Sigmoid),
                     reads=[f'B{2 + par}'], writes=[f'sg2{par}'])
                S.op('dve', lambda e: e.tensor_tensor(out=sg1[par], in0=sg1[par], in1=B[4 + par], op=ALU.mult),
                     reads=[f'sg1{par}', f'B{4 + par}'], writes=[f'sg1{par}'])
                S.op('dve', lambda e: e.tensor_tensor(out=sg2[par], in0=sg2[par], in1=B[6 + par], op=ALU.mult),
                     reads=[f'sg2{par}', f'B{6 + par}'], writes=[f'sg2{par}'])
                S.op('dve', lambda e: e.tensor_tensor(out=mergedT[:, fcx, ts_], in0=sg1[par], in1=sg2[par], op=ALU.add),
                     reads=[f'sg1{par}', f'sg2{par}'], writes=['mergedT'])
        if u + 2 < 8:
            load_u4(u + 2)
    dbg("mergedT", mergedT, [128, 16, 1024], BF16, ['mergedT'])

    x1 = v3(A.f32(8 * KB, 8 * 2048), 8, 2048)
    wo = [v3(A.bf16(108 * KB + i * 16 * KB, 8192), 16, 512) for i in range(2)]
    S.barrier()
    for tt in range(8):
        S.dma('sp', x1[:, tt, :], xc[7168 + tt * 128:7168 + (tt + 1) * 128, :], f'x1_{tt}',
              writes=[f'x1_{tt}'])
    def load_wo(cb):
        s = cb % 2
        S.dma('pool', wo[s], w_out[:, cb * 512:(cb + 1) * 512].rearrange("(c p) f -> p c f", p=128), f'wo{s}',
              writes=[f'wo{s}'])

    load_wo(0)
    load_wo(1)
    for cb in range(4):
        s = cb % 2
        for tt in range(8):
            pb = tt % 4
            for fc in range(16):
                S.op('pe', lambda e: e.matmul(B[pb], lhsT=mergedT[:, fc, tt * 128:(tt + 1) * 128], rhs=wo[s][:, fc, :],
                                              start=(fc == 0), stop=(fc == 15)),
                     reads=['mergedT', f'wo{s}'], writes=[f'B{pb}'], inc=(fc == 15))
            S.op('dve', lambda e: e.tensor_tensor(out=x1[:, tt, cb * 512:(cb + 1) * 512], in0=B[pb],
                                                  in1=x1[:, tt, cb * 512:(cb + 1) * 512], op=ALU.add),
                 reads=[f'B{pb}', f'x1_{tt}'], writes=[f'x1_{tt}'])
        if cb + 2 < 4:
            load_wo(cb + 2)
    x1k = [f'x1_{tt}' for tt in range(8)]
    dbg("x1", x1, [128, 8, 2048], F32, x1k)
    if stop_after <= 4:
        return finish()

    h2T = v3(A.bf16(72 * KB, 16384), 16, 1024)
    hidT = v3(A.bf16(104 * KB, 8192), 8, 1024)
    ring = [A.bf16(120 * KB + i * 8 * KB, 4096) for i in range(8)]
    h2f = A.f32(184 * KB, 2048)
    h2hi = A.bf16(104 * KB, 2048)
    h2lo = A.bf16(108 * KB, 2048)
    h2Tlo = v3(A.bf16(112 * KB, 2048), 16, 128)
    gbc = A.f32(194 * KB, 2048)
    o = 1792

    wr = v3(cf32(320), 16, 20)
    wrhi = v3(cbf(320), 16, 20)
    wrlo = v3(cbf(320), 16, 20)
    assert o <= 4420, o
    o = 192 * KB
    comb = v3(cf32(128), 8, 16)
    lg = cf32(20)
    goh = cf32(4)
    esel = cf32(4)
    esel2 = cf32(4)
    oh1 = cf32(4)
    oh2 = cf32(4)
    ce = cf32(4)
    sc = cf32(16)
    ge = cf32(4)
    sa = [A.f32(202 * KB + i * 2 * KB, 512) for i in range(2)]
    assert o <= 194 * KB, o
    S.barrier()
    S.dma('sp', wr, v3(wr_d, 16, 20), 'wr', writes=['wr'])
    S.op('dve', lambda e: e.tensor_copy(out=wrhi, in_=wr), reads=['wr'], writes=['wrhi'])
    S.op('dve', lambda e: e.scalar_tensor_tensor(out=wrlo, in0=wrhi, scalar=-1.0, in1=wr, op0=ALU.mult, op1=ALU.add), reads=['wr', 'wrhi'], writes=['wrlo'])
    S.dma('sp', gbc, g2_d, 'gbc', writes=['gbc'])
    units = []
    for ex in range(16):
        for q in range(4):
            units.append(('g', ex, q))
            units.append(('u', ex, q))
        for q in range(4):
            units.append(('d', ex, q))
    NU = len(units)

    def load_unit(k):
        kind, ex, q = units[k]
        s = k % 8
        if kind == 'd':
            dst = v3(ring[s], 2, 2048)
            src = w_down[ex, q * 256:(q + 1) * 256, :].rearrange("(c p) d -> p c d", p=128)
        else:
            wsrc = w_gate if kind == 'g' else w_up
            dst = v3(ring[s], 16, 256)
            src = wsrc[ex, :, q * 256:(q + 1) * 256].rearrange("(c p) f -> p c f", p=128)
        S.dma('pool', dst, src, f'ring{s}', writes=[f'ring{s}'])

    PRE = 7
    for k in range(PRE):
        load_unit(k)
    nextload = PRE
    S.op('dve', lambda e: e.memset(ssq, 0.0), reads=['ssq'] + [f'ssq{i}' for i in range(64)], writes=['ssq2'])
    for tt in range(8):
        S.op('act', lambda e: e.activation(out=h2f, in_=x1[:, tt, :], func=AF.Square, accum_out=ssq[:, tt:tt + 1]),
             reads=[f'x1_{tt}', 'ssq2'], writes=['h2f', f'ssqb{tt}'])
        S.op('act', lambda e: e.activation(out=rstd[:, tt:tt + 1], in_=ssq[:, tt:tt + 1], func=AF.Sqrt,
                                           scale=1.0 / 2048, bias=epst), reads=[f'ssqb{tt}', 'epst'], writes=[f'rstdb{tt}'])
        S.op('dve', lambda e: e.reciprocal(out=rstd[:, tt:tt + 1], in_=rstd[:, tt:tt + 1]),
             reads=[f'rstdb{tt}'], writes=[f'rstdb{tt}'])
        S.op('dve', lambda e: e.scalar_tensor_tensor(out=h2f, in0=x1[:, tt, :], scalar=rstd[:, tt:tt + 1], in1=gbc,
                                                     op0=ALU.mult, op1=ALU.mult),
             reads=[f'x1_{tt}', f'rstdb{tt}', 'gbc', 'h2f'], writes=['h2f'])
        S.op('dve', lambda e: e.tensor_copy(out=h2hi, in_=h2f), reads=['h2f'], writes=['h2hi'])
        S.op('dve', lambda e: e.scalar_tensor_tensor(out=h2lo, in0=h2hi, scalar=-1.0, in1=h2f, op0=ALU.mult, op1=ALU.add), reads=['h2f', 'h2hi'],
             writes=['h2lo'])
        for half in range(2):
            for k8 in range(8):
                dc = half * 8 + k8
                S.op('pe', lambda e: e.transpose(Bb[half][:, k8 * 128:(k8 + 1) * 128], h2hi[:, dc * 128:(dc + 1) * 128],
                                                 ident_b), reads=['h2hi', 'ident_b'], writes=[f'B{half}'], inc=(k8 == 7))
            S.op('act', lambda e: e.activation(out=h2T[:, half * 8:(half + 1) * 8, tt * 128:(tt + 1) * 128],
                                               in_=v3(Bb[half], 8, 128), func=AF.Copy),
                 reads=[f'B{half}'], writes=['h2T'])
        for half in range(2):
            for k8 in range(8):
                dc = half * 8 + k8
                S.op('pe', lambda e: e.transpose(Bb[2 + half][:, k8 * 128:(k8 + 1) * 128],
                                                 h2lo[:, dc * 128:(dc + 1) * 128], ident_b),
                     reads=['h2lo', 'ident_b'], writes=[f'B{2 + half}'], inc=(k8 == 7))
            S.op('act', lambda e: e.activation(out=h2Tlo[:, half * 8:(half + 1) * 8, :], in_=v3(Bb[2 + half], 8, 128),
                                               func=AF.Copy), reads=[f'B{2 + half}'], writes=['h2Tlo'])
        rk = ['h2T', 'h2Tlo', 'wrhi', 'wrlo']
        for dc in range(16):
            hsl = h2T[:, dc, tt * 128:(tt + 1) * 128]
            S.op('pe', lambda e: e.matmul(B[6][:, 0:20], lhsT=hsl, rhs=wrhi[:, dc, :], start=(dc == 0), stop=False),
                 reads=rk, writes=['B6'], inc=False)
            S.op('pe', lambda e: e.matmul(B[6][:, 0:20], lhsT=hsl, rhs=wrlo[:, dc, :], start=False, stop=False),
                 reads=rk, writes=['B6'], inc=False)
            S.op('pe', lambda e: e.matmul(B[6][:, 0:20], lhsT=h2Tlo[:, dc, :], rhs=wrhi[:, dc, :], start=False,
                                          stop=(dc == 15)), reads=rk, writes=['B6'], inc=(dc == 15))
        S.op('dve', lambda e: e.tensor_tensor(out=lg, in0=B[6][:, 0:20], in1=br, op=ALU.add),
             reads=['B6', 'br'], writes=['lg'])
        S.op('dve', lambda e: e.tensor_reduce(out=sc[:, 0:1], in_=lg[:, 0:4], axis=AX.X, op=ALU.max),
             reads=['lg'], writes=['sc0'])
        S.op('dve', lambda e: e.tensor_scalar(out=goh, in0=lg[:, 0:4], scalar1=sc[:, 0:1], scalar2=None, op0=ALU.is_ge),
             reads=['lg', 'sc0'], writes=['goh'])
        S.op('dve', lambda e: e.tensor_scalar(out=sc[:, 1:2], in0=sc[:, 0:1], scalar1=-1.0, scalar2=None, op0=ALU.mult),
             reads=['sc0'], writes=['sc1'])
        S.op('dve', lambda e: e.memset(sc[:, 2:3], 0.0), writes=['sc2'])
        S.op('act', lambda e: e.activation(out=ge, in_=lg[:, 0:4], func=AF.Exp, bias=sc[:, 1:2], accum_out=sc[:, 2:3]),
             reads=['lg', 'sc1', 'sc2'], writes=['ge', 'sc2'])
        S.op('dve', lambda e: e.reciprocal(out=sc[:, 3:4], in_=sc[:, 2:3]), reads=['sc2'], writes=['sc3'])
        S.op('dve', lambda e: e.tensor_scalar(out=esel, in0=lg[:, 4:8], scalar1=goh[:, 0:1], scalar2=None, op0=ALU.mult),
             reads=['lg', 'goh'], writes=['esel'])
        for g in range(1, 4):
            S.op('dve', lambda e: e.scalar_tensor_tensor(out=esel, in0=lg[:, 4 + 4 * g:8 + 4 * g], scalar=goh[:, g:g + 1],
                                                         in1=esel, op0=ALU.mult, op1=ALU.add),
                 reads=['lg', 'goh', 'esel'], writes=['esel'])
        S.op('dve', lambda e: e.tensor_reduce(out=sc[:, 4:5], in_=esel, axis=AX.X, op=ALU.max),
             reads=['esel'], writes=['sc4'])
        S.op('dve', lambda e: e.tensor_scalar(out=oh1, in0=esel, scalar1=sc[:, 4:5], scalar2=None, op0=ALU.is_ge),
             reads=['esel', 'sc4'], writes=['oh1'])
        S.op('dve', lambda e: e.scalar_tensor_tensor(out=esel2, in0=oh1, scalar=NEG, in1=esel, op0=ALU.mult, op1=ALU.add),
             reads=['oh1', 'esel'], writes=['esel2'])
        S.op('dve', lambda e: e.tensor_reduce(out=sc[:, 5:6], in_=esel2, axis=AX.X, op=ALU.max),
             reads=['esel2'], writes=['sc5'])
        S.op('dve', lambda e: e.tensor_scalar(out=oh2, in0=esel2, scalar1=sc[:, 5:6], scalar2=None, op0=ALU.is_ge),
             reads=['esel2', 'sc5'], writes=['oh2'])
        S.op('dve', lambda e: e.tensor_tensor(out=sc[:, 6:7], in0=sc[:, 5:6], in1=sc[:, 4:5], op=ALU.subtract),
             reads=['sc4', 'sc5'], writes=['sc6'])
        S.op('act', lambda e: e.activation(out=sc[:, 7:8], in_=sc[:, 6:7], func=AF.Exp), reads=['sc6'], writes=['sc7'])
        S.op('dve', lambda e: e.tensor_scalar(out=sc[:, 8:9], in0=sc[:, 7:8], scalar1=1.0, scalar2=None, op0=ALU.add),
             reads=['sc7'], writes=['sc8'])
        S.op('dve', lambda e: e.reciprocal(out=sc[:, 9:10], in_=sc[:, 8:9]), reads=['sc8'], writes=['sc9'])
        S.op('dve', lambda e: e.tensor_tensor(out=sc[:, 10:11], in0=sc[:, 7:8], in1=sc[:, 9:10], op=ALU.mult),
             reads=['sc7', 'sc9'], writes=['sc10'])
        S.op('dve', lambda e: e.tensor_tensor(out=sc[:, 11:12], in0=sc[:, 9:10], in1=sc[:, 3:4], op=ALU.mult),
             reads=['sc9', 'sc3'], writes=['sc11'])
        S.op('dve', lambda e: e.tensor_tensor(out=sc[:, 12:13], in0=sc[:, 10:11], in1=sc[:, 3:4], op=ALU.mult),
             reads=['sc10', 'sc3'], writes=['sc12'])
        S.op('dve', lambda e: e.tensor_scalar(out=ce, in0=oh1, scalar1=sc[:, 11:12], scalar2=None, op0=ALU.mult),
             reads=['oh1', 'sc11'], writes=['ce'])
        S.op('dve', lambda e: e.scalar_tensor_tensor(out=ce, in0=oh2, scalar=sc[:, 12:13], in1=ce, op0=ALU.mult,
                                                     op1=ALU.add), reads=['oh2', 'sc12', 'ce'], writes=['ce'])
        for g in range(4):
            S.op('dve', lambda e: e.tensor_scalar(out=comb[:, tt, 4 * g:4 * g + 4], in0=ce, scalar1=goh[:, g:g + 1],
                                                  scalar2=None, op0=ALU.mult), reads=['ce', 'goh'], writes=['comb'])
    dbg("comb", comb, [128, 8, 16], F32, ['comb'])
    dbg("h2T", h2T, [128, 16, 1024], BF16, ['h2T'])

    k = 0
    ai = di = 0
    for ex in range(16):
        dslots = []
        for q in range(4):
            gs, us = k % 8, (k + 1) % 8
            G = v3(ring[gs], 16, 256)
            U = v3(ring[us], 16, 256)
            for fl in range(2):
                fc = 2 * q + fl
                fs = slice(fl * 128, (fl + 1) * 128)
                for th in range(2):
                    par = ai % 2
                    ai += 1
                    ts_ = slice(th * 512, (th + 1) * 512)
                    for dc in range(16):
                        S.op('pe', lambda e: e.matmul(B[par], lhsT=G[:, dc, fs], rhs=h2T[:, dc, ts_], start=(dc == 0),
                                                      stop=(dc == 15)),
                             reads=[f'ring{gs}', 'h2T'], writes=[f'B{par}'], inc=(dc == 15))
                    for dc in range(16):
                        S.op('pe', lambda e: e.matmul(B[2 + par], lhsT=U[:, dc, fs], rhs=h2T[:, dc, ts_], start=(dc == 0),
                                                      stop=(dc == 15)),
                             reads=[f'ring{us}', 'h2T'], writes=[f'B{2 + par}'], inc=(dc == 15))
                    S.op('act', lambda e: e.activation(out=sa[par], in_=B[par], func=AF.Silu),
                         reads=[f'B{par}'], writes=[f'sa{par}'])
                    S.op('dve', lambda e: e.tensor_tensor(out=hidT[:, fc, ts_], in0=sa[par], in1=B[2 + par], op=ALU.mult),
                         reads=[f'sa{par}', f'B{2 + par}'], writes=['hidT', 'h2hi', 'h2lo', 'h2Tlo'])
            k += 2
            while nextload < min(NU, k + PRE):
                load_unit(nextload)
                nextload += 1
        for q in range(4):
            dslots.append(k % 8)
            k += 1
        for tt in range(8):
            for cb in range(4):
                po = 4 + di % 3
                di += 1
                for fc in range(8):
                    ds = dslots[fc // 2]
                    D = v3(ring[ds], 2, 2048)
                    S.op('pe', lambda e: e.matmul(B[po], lhsT=hidT[:, fc, tt * 128:(tt + 1) * 128],
                                                  rhs=D[:, fc % 2, cb * 512:(cb + 1) * 512], start=(fc == 0),
                                                  stop=(fc == 7)),
                         reads=['hidT', f'ring{ds}'], writes=[f'B{po}'], inc=(fc == 7))
                S.op('dve', lambda e: e.scalar_tensor_tensor(out=x1[:, tt, cb * 512:(cb + 1) * 512], in0=B[po],
                                                             scalar=comb[:, tt, ex:ex + 1],
                                                             in1=x1[:, tt, cb * 512:(cb + 1) * 512], op0=ALU.mult,
                                                             op1=ALU.add),
                     reads=[f'B{po}', 'comb', f'x1_{tt}'], writes=[f'x1_{tt}'])
        while nextload < min(NU, k + PRE):
            load_unit(nextload)
            nextload += 1

    S.barrier()
    S.dma('sp', gbc, g3_d, 'gbc', writes=['gbc'])
    S.op('dve', lambda e: e.memset(ssq, 0.0), reads=[f'ssqb{tt}' for tt in range(8)], writes=['ssq3'])
    outs = []
    ob = [h2f, A.f32(120 * KB, 2048)]
    for tt in range(8):
        oo = ob[tt % 2]
        okey = f'obuf{tt % 2}'
        extra = [f'ring{s}' for s in range(8)] if tt < 2 else []
        S.op('act', lambda e: e.activation(out=oo, in_=x1[:, tt, :], func=AF.Square, accum_out=ssq[:, tt:tt + 1]),
             reads=[f'x1_{tt}', 'ssq3', 'h2f'], writes=[okey, f'ssqc{tt}'] + extra)
        S.op('act', lambda e: e.activation(out=rstd[:, tt:tt + 1], in_=ssq[:, tt:tt + 1], func=AF.Sqrt,
                                           scale=1.0 / 2048, bias=epst), reads=[f'ssqc{tt}', f'rstdb{tt}', 'epst'], writes=[f'rstdc{tt}'])
        S.op('dve', lambda e: e.reciprocal(out=rstd[:, tt:tt + 1], in_=rstd[:, tt:tt + 1]),
             reads=[f'rstdc{tt}'], writes=[f'rstdc{tt}'])
        S.op('dve', lambda e: e.scalar_tensor_tensor(out=oo, in0=x1[:, tt, :], scalar=rstd[:, tt:tt + 1], in1=gbc,
                                                     op0=ALU.mult, op1=ALU.mult),
             reads=[f'x1_{tt}', f'rstdc{tt}', 'gbc', okey], writes=[okey])
        outs.append(S.dma('sp', out_d[tt * 128:(tt + 1) * 128, :], oo, f'out{tt % 2}', reads=[okey]))
    return finish(outs)


_NC_CACHE = {}


def _prep_inputs(x, norm_mix, w_in, w_pool, pool_scale, w_branch_pool, w_branch_attn, w_out, norm_ffn, w_r_group,
                 b_r_group, w_r_expert, b_r_expert, w_gate, w_up, w_down, norm_final):
    f = np.float32
    x2 = np.asarray(x, f)[0]
    bc = lambda v: np.ascontiguousarray(np.broadcast_to(np.asarray(v, f).reshape(1, -1), (128, np.asarray(v).size)))
    wr = np.concatenate([np.asarray(w_r_group, f)[0]] + [np.asarray(w_r_expert, f)[0][g] for g in range(4)], axis=1)
    wr = np.ascontiguousarray(wr.reshape(16, 128, 20).transpose(1, 0, 2).reshape(128, 320))
    brv = np.concatenate([np.asarray(b_r_group, f)[0], np.asarray(b_r_expert, f)[0].reshape(16)])
    common = {
        "w_in": np.ascontiguousarray(np.asarray(w_in, f)[0]),
        "w_pool": np.ascontiguousarray(np.asarray(w_pool, f)[0]),
        "w_bp": np.ascontiguousarray(np.asarray(w_branch_pool, f)[0]),
        "w_ba": np.ascontiguousarray(np.asarray(w_branch_attn, f)[0]),
        "w_out": np.ascontiguousarray(np.asarray(w_out, f)[0]),
        "w_gate": np.ascontiguousarray(np.asarray(w_gate, f)[0]),
        "w_up": np.ascontiguousarray(np.asarray(w_up, f)[0]),
        "w_down": np.ascontiguousarray(np.asarray(w_down, f)[0]),
        "g1": bc(np.asarray(norm_mix)[0]), "g2": bc(np.asarray(norm_ffn)[0]), "g3": bc(norm_final),
        "pst": np.ascontiguousarray(np.asarray(pool_scale, f)[0].reshape(8, 128).T),
        "wr": wr, "br": bc(brv),
    }
    in_maps = []
    for c in range(NCORES):
        n = 1024 * (c + 1)
        xcx = np.zeros((8192, 2048), f)
        xcx[8192 - n:] = x2[:n]
        pm = np.zeros((128, 32), f)
        pm[:, :28 - 4 * c] = NEG
        ic = np.zeros((128, 8, 16), f)
        for ch in range(8):
            w = 2 ** (ch // 2 + 1)
            for t in range(16):
                ic[:, ch, t] = 1.0 / min(1024 * c + t + 1, w)
        m = dict(common)
        m["xc"] = xcx
        m["padmask"] = pm
        m["invcnt"] = ic.reshape(128, 128)
        in_maps.append(m)
    return in_maps


def kernel(**inputs):
    in_maps = _prep_inputs(**inputs)
    if "nc" not in _NC_CACHE:
        _NC_CACHE["nc"] = build()
    nc = _NC_CACHE["nc"]
    res = run_bass_kernel_spmd(nc, in_maps, core_ids=list(range(NCORES)))
    out = np.concatenate([np.asarray(r["out"], np.float32) for r in res.results], axis=0)
    return out.reshape(1, 8192, 2048)
```

```python
import numpy as np
import concourse.bass as bass
import concourse.mybir as mybir
from concourse.bass_utils import run_bass_kernel_spmd

F32 = mybir.dt.float32
BF16 = mybir.dt.bfloat16
I32 = mybir.dt.int32
AF = mybir.ActivationFunctionType
ALU = mybir.AluOpType
AX = mybir.AxisListType

NEG = -1.0e30
SLOPES = [2.0 ** (-(h + 1)) for h in range(8)]
SCALE = 128.0 ** -0.5
EPS = 1e-6
NCORES = 8
KB = 1024


class Sched:
    def __init__(self, nc):
        self.nc = nc
        self.eng = {'pe': nc.tensor, 'act': nc.scalar, 'dve': nc.vector, 'pool': nc.gpsimd, 'sp': nc.sync}
        self.esem = {k: nc.alloc_semaphore(name=f"s_{k}") for k in ['pe', 'act', 'dve', 'pool']}
        self.ecnt = {k: 0 for k in self.esem}
        self.seen = {k: {} for k in self.eng}
        self.lastw = {}
        self.reads = {}
        self.dsem = {}
        self.dcnt = {}

    def _wait(self, e, tok):
        sem, val, src, sid = tok
        if src == e and e == 'pe':
            return
        if self.seen[e].get(sid, 0) >= val:
            return
        self.eng[e].wait_ge(sem, val)
        self.seen[e][sid] = val

    def _deps(self, e, reads, writes):
        for r in reads:
            if r in self.lastw:
                self._wait(e, self.lastw[r])
        for w in writes:
            if w in self.lastw:
                self._wait(e, self.lastw[w])
            for t in self.reads.get(w, {}).values():
                self._wait(e, t)

    def _commit(self, tok, reads, writes):
        for r in reads:
            self.reads.setdefault(r, {})[tok[3]] = tok
        for w in writes:
            self.lastw[w] = tok
            self.reads[w] = {}

    def op(self, e, fn, reads=(), writes=(), inc=True):
        self._deps(e, reads, writes)
        ins = fn(self.eng[e])
        if inc:
            self.ecnt[e] += 1
            ins.then_inc(self.esem[e], 1)
            tok = (self.esem[e], self.ecnt[e], e, 'E' + e)
        else:
            tok = (self.esem[e], self.ecnt[e] + 1, e, 'E' + e)
        self._commit(tok, reads, writes)
        return tok

    def dma(self, e, out, in_, semkey, reads=(), writes=()):
        if semkey not in self.dsem:
            self.dsem[semkey] = self.nc.alloc_semaphore(name=f"d_{semkey}")
            self.dcnt[semkey] = 0
        self._deps(e, reads, writes)
        ins = self.eng[e].dma_start(out=out, in_=in_)
        self.dcnt[semkey] += 16
        ins.then_inc(self.dsem[semkey], 16)
        tok = (self.dsem[semkey], self.dcnt[semkey], 'dma', 'D' + semkey)
        self._commit(tok, reads, writes)
        return tok

    def wait_tok(self, e, tok):
        self._wait(e, tok)

    def barrier(self):
        for e in self.eng:
            for e2, sem in self.esem.items():
                if self.ecnt[e2] > 0:
                    self._wait(e, (sem, self.ecnt[e2], e2, 'E' + e2))
            for k, sem in self.dsem.items():
                if self.dcnt[k] > 0:
                    self._wait(e, (sem, self.dcnt[k], 'dma', 'D' + k))


def v3(ap, a, b):
    return ap.rearrange("p (a b) -> p a b", a=a, b=b)


class Arena:
    def __init__(self, nc, nbytes):
        self.t = nc.alloc_sbuf_tensor("arena", [128, nbytes // 4], F32)
        self.nbytes = nbytes

    def f32(self, off, n):
        assert off % 4 == 0 and off + 4 * n <= self.nbytes, (off, n)
        return self.t[:, off // 4: off // 4 + n]

    def bf16(self, off, n):
        assert off % 4 == 0 and n % 2 == 0 and off + 2 * n <= self.nbytes, (off, n)
        return self.t[:, off // 4: off // 4 + n // 2].bitcast(BF16)


def build(stop_after=99, debug=False):
    nc = bass.Bass("TRN2", target_bir_lowering=False)

    def din(name, shape, dtype=F32):
        return nc.dram_tensor(name, list(shape), dtype, kind="ExternalInput").ap()

    xc = din("xc", [8192, 2048])
    w_in = din("w_in", [2048, 8192])
    w_pool = din("w_pool", [4, 256, 256])
    w_bp = din("w_bp", [1024, 2048])
    w_ba = din("w_ba", [1024, 2048])
    w_out = din("w_out", [2048, 2048])
    w_gate = din("w_gate", [16, 2048, 1024])
    w_up = din("w_up", [16, 2048, 1024])
    w_down = din("w_down", [16, 1024, 2048])
    g1_d = din("g1", [128, 2048])
    g2_d = din("g2", [128, 2048])
    g3_d = din("g3", [128, 2048])
    pst_d = din("pst", [128, 8])
    wr_d = din("wr", [128, 16 * 20])
    br_d = din("br", [128, 20])
    padmask_d = din("padmask", [128, 32])
    invcnt_d = din("invcnt", [128, 8 * 16])
    out_d = nc.dram_tensor("out", [1024, 2048], F32, kind="ExternalOutput").ap()
    kt_d = nc.dram_tensor("kt_scr", [8, 128, 8192], BF16, kind="Internal").ap()
    vs_d = nc.dram_tensor("vs_scr", [8192, 1024], BF16, kind="Internal").ap()

    dbg_outs = []

    S = Sched(nc)
    A = Arena(nc, 206 * KB)
    banks = [nc.alloc_psum_tensor(f"bank{i}", [128, 512], F32) for i in range(8)]
    B = [b[:, :] for b in banks]
    Bb = [b[:, :].bitcast(BF16) for b in banks]

    def dbg(name, ap, shape, dtype, reads):
        if not debug:
            return
        d = nc.dram_tensor("dbg_" + name, list(shape), dtype, kind="ExternalOutput").ap()
        dbg_outs.append(S.dma('sp', d, ap, 'dbg_' + name, reads=reads))

    def finish(extra=()):
        for t in list(extra) + dbg_outs:
            S.wait_tok('sp', t)
        return nc

    o = 0

    def cf32(n):
        nonlocal o
        ap = A.f32(o, n)
        o += 4 * n
        return ap

    def cbf(n):
        nonlocal o
        ap = A.bf16(o, n)
        o += 2 * n
        return ap

    ident_b = cbf(128)
    tri = cbf(128)
    ones_b = cbf(128)
    ones_sq = ones_b
    ident_f = cf32(128)
    ones_f = cf32(128)
    ksum = v3(cf32(256), 8, 32)
    padmask = cf32(32)
    invcnt = v3(cf32(128), 8, 16)
    pst = cf32(8)
    kbias = cf32(8)
    iop = cf32(1)
    Dall = v3(cf32(224), 8, 28)
    Dnr = cf32(8)
    NWr = [cf32(8) for _ in range(2)]
    ssq = cf32(64)
    rstd = cf32(64)
    m8 = cf32(8)
    selt = cf32(32)
    rden = cf32(2)
    br = cf32(20)
    epst = cf32(1)
    itmp = A.t[:, o // 4: o // 4 + 224].bitcast(I32)
    o += 4 * 224
    assert o <= 8 * KB, o

    S.op('pool', lambda e: e.memset(ones_b, 1.0), writes=['ones_b'])
    S.op('pool', lambda e: e.memset(ones_f, 1.0), writes=['ones_f'])
    S.op('pool', lambda e: e.affine_select(out=ident_b, in_=ones_b, pattern=[[-1, 128]], compare_op=ALU.is_equal,
                                           fill=0.0, base=0, channel_multiplier=1), reads=['ones_b'], writes=['ident_b'])
    S.op('pool', lambda e: e.affine_select(out=ident_f, in_=ones_f, pattern=[[-1, 128]], compare_op=ALU.is_equal,
                                           fill=0.0, base=0, channel_multiplier=1), reads=['ones_f'], writes=['ident_f'])
    S.op('pool', lambda e: e.affine_select(out=tri, in_=ones_b, pattern=[[1, 128]], compare_op=ALU.is_ge,
                                           fill=0.0, base=0, channel_multiplier=-1), reads=['ones_b'], writes=['tri'])
    S.op('pool', lambda e: e.iota(v3(itmp[:, 0:224], 8, 28), pattern=[[128, 8], [-256, 28]], base=6913, channel_multiplier=1),
         writes=['itmp'])
    S.op('dve', lambda e: e.tensor_copy(out=Dall, in_=v3(itmp[:, 0:224], 8, 28)), reads=['itmp'], writes=['Dall'])
    S.op('pool', lambda e: e.iota(itmp[:, 0:8], pattern=[[-128, 8]], base=769, channel_multiplier=1),
         reads=['Dall'], writes=['itmp'])
    S.op('dve', lambda e: e.tensor_copy(out=Dnr, in_=itmp[:, 0:8]), reads=['itmp'], writes=['Dnr'])
    S.op('pool', lambda e: e.iota(itmp[:, 0:1], pattern=[[0, 1]], base=-127, channel_multiplier=1),
         reads=['Dnr'], writes=['itmp'])
    S.op('dve', lambda e: e.tensor_copy(out=iop, in_=itmp[:, 0:1]), reads=['itmp'], writes=['iop'])
    for h in range(8):
        S.op('dve', lambda e: e.tensor_scalar(out=kbias[:, h:h + 1], in0=iop, scalar1=SLOPES[h], scalar2=None,
                                              op0=ALU.mult), reads=['iop'], writes=['kbias'])
    S.op('dve', lambda e: e.memset(ksum, 0.0), writes=['ksum'])
    S.op('dve', lambda e: e.memset(epst, EPS), writes=['epst'])
    S.op('dve', lambda e: e.memset(ssq, 0.0), writes=['ssq'])
    S.dma('sp', padmask, padmask_d, 'c0', writes=['padmask'])
    S.dma('sp', invcnt, v3(invcnt_d, 8, 16), 'c1', writes=['invcnt'])
    S.dma('sp', pst, pst_d, 'c2', writes=['pst'])
    S.dma('sp', br, br_d, 'c3', writes=['br'])

    hT = [v3(A.bf16(8 * KB + i * 16 * KB, 8192), 16, 512) for i in range(2)]
    hTh = v3(A.bf16(40 * KB, 2048), 16, 128)
    W = [v3(A.bf16(44 * KB + i * 16 * KB, 8192), 16, 512) for i in range(4)]
    g1bc = A.f32(108 * KB, 2048)
    xs = [A.f32(116 * KB + i * 8 * KB, 2048) for i in range(3)]
    xb = [A.bf16(140 * KB + i * 4 * KB, 2048) for i in range(2)]
    kst = [v3(A.bf16(148 * KB + i * 8 * KB, 4096), 8, 512) for i in range(2)]
    vst = [v3(A.bf16(164 * KB + i * 8 * KB, 4096), 4, 1024) for i in range(2)]
    junk = A.bf16(180 * KB, 2048)

    def load_w_in(q, c0, key):
        return S.dma('pool', W[q], w_in[:, c0:c0 + 512].rearrange("(c p) f -> p c f", p=128), key, writes=[key])

    S.dma('sp', g1bc, g1_d, 'g1bc', writes=['g1bc'])
    for q in range(4):
        load_w_in(q, 2048 + q * 512, f'W{q}')

    store_toks = []
    for g in range(16):
        hs = g % 2
        for t in range(4):
            i = 4 * g + t
            xi, bi = i % 3, i % 2
            S.dma('sp', xs[xi], xc[i * 128:(i + 1) * 128, :], f'xs{xi}', writes=[f'xs{xi}'])
            S.op('act', lambda e: e.activation(out=junk, in_=xs[xi], func=AF.Square, accum_out=ssq[:, i:i + 1]),
                 reads=[f'xs{xi}', 'ssq'], writes=['junk', f'ssq{i}'])
            S.op('act', lambda e: e.activation(out=rstd[:, i:i + 1], in_=ssq[:, i:i + 1], func=AF.Sqrt,
                                               scale=1.0 / 2048, bias=epst), reads=[f'ssq{i}', 'epst'], writes=[f'rstd{i}'])
            S.op('dve', lambda e: e.reciprocal(out=rstd[:, i:i + 1], in_=rstd[:, i:i + 1]),
                 reads=[f'rstd{i}'], writes=[f'rstd{i}'])
            S.op('dve', lambda e: e.scalar_tensor_tensor(out=xb[bi], in0=xs[xi], scalar=rstd[:, i:i + 1], in1=g1bc,
                                                         op0=ALU.mult, op1=ALU.mult),
                 reads=[f'xs{xi}', f'rstd{i}', 'g1bc'], writes=[f'xb{bi}'])
            for half in range(2):
                bk = (i % 2) * 2 + half
                for k in range(8):
                    dc = half * 8 + k
                    S.op('pe', lambda e: e.transpose(Bb[bk][:, k * 128:(k + 1) * 128], xb[bi][:, dc * 128:(dc + 1) * 128],
                                                     ident_b),
                         reads=[f'xb{bi}', 'ident_b'], writes=[f'B{bk}'], inc=(k == 7))
                S.op('act', lambda e: e.activation(out=hT[hs][:, half * 8:(half + 1) * 8, t * 128:(t + 1) * 128],
                                                   in_=v3(Bb[bk], 8, 128), func=AF.Copy),
                     reads=[f'B{bk}'], writes=[f'hT{hs}_{t}_{half}'])
        hkeys = [f'hT{hs}_{t}_{half}' for t in range(4) for half in range(2)]
        if g == 13:
            S.op('dve', lambda e: e.tensor_copy(out=hTh, in_=hT[hs][:, :, 384:512]), reads=hkeys, writes=['hTh'])
        ks = g % 2
        for h in range(8):
            pk = 4 + h % 2
            for dc in range(16):
                S.op('pe', lambda e: e.matmul(B[pk], lhsT=W[h // 4][:, dc, (h % 4) * 128:(h % 4 + 1) * 128],
                                              rhs=hT[hs][:, dc, :], start=(dc == 0), stop=(dc == 15)),
                     reads=[f'W{h // 4}'] + hkeys, writes=[f'B{pk}'], inc=(dc == 15))
            for half in range(2):
                S.op('act', lambda e: e.activation(out=kst[ks][:, h, half * 256:(half + 1) * 256],
                                                   in_=B[pk][:, half * 256:(half + 1) * 256], func=AF.Copy,
                                                   accum_out=ksum[:, h, 2 * g + half:2 * g + half + 1]),
                     reads=[f'B{pk}', 'ksum'], writes=[f'kst{ks}', f'ksum{g}'])
        store_toks.append(S.dma('pool', kt_d[:, :, g * 512:(g + 1) * 512].rearrange("h p t -> p h t"), kst[ks],
                                f'kstS{ks}', reads=[f'kst{ks}']))
        vsl = g % 2
        for t in range(4):
            for cb in range(2):
                pv = 6 + (t * 2 + cb) % 2
                for dc in range(16):
                    S.op('pe', lambda e: e.matmul(B[pv], lhsT=hT[hs][:, dc, t * 128:(t + 1) * 128],
                                                  rhs=W[2 + cb][:, dc, :], start=(dc == 0), stop=(dc == 15)),
                         reads=[f'W{2 + cb}'] + hkeys, writes=[f'B{pv}'], inc=(dc == 15))
                S.op('dve', lambda e: e.tensor_copy(out=vst[vsl][:, t, cb * 512:(cb + 1) * 512], in_=B[pv]),
                     reads=[f'B{pv}'], writes=[f'vst{vsl}'])
        store_toks.append(S.dma('pool', vs_d[g * 512:(g + 1) * 512, :].rearrange("(t p) d -> p t d", p=128), vst[vsl],
                                f'vstS{vsl}', reads=[f'vst{vsl}']))
    hk = [[f'hT{hs}_{t}_{half}' for t in range(4) for half in range(2)] for hs in range(2)]
    dbg("ksum", ksum, [128, 8, 32], F32, ['ksum'] + [f'ksum{g}' for g in range(16)])
    dbg("rstd", rstd, [128, 64], F32, [f'rstd{i}' for i in range(64)])
    dbg("hT0", hT[0], [128, 16, 512], BF16, hk[0])
    if stop_after <= 1:
        return finish(store_toks)

    uT = v3(A.f32(108 * KB, 8 * 1040), 8, 1040)
    tA = v3(A.f32(141 * KB, 2080), 2, 1040)
    tB = v3(A.f32(150 * KB, 2080), 2, 1040)
    mixT = v3(A.bf16(159 * KB, 8192), 8, 1024)
    wpool = A.bf16(175 * KB, 2048).rearrange("p (g i o) -> p g i o", g=4, i=2, o=256)
    qhi = [A.bf16(179 * KB + i * KB, 512) for i in range(2)]
    qlo = [A.bf16(181 * KB + i * KB, 512) for i in range(2)]
    kshi = v3(A.bf16(192 * KB, 256), 8, 32)
    kslo = v3(A.bf16(192 * KB + 512, 256), 8, 32)
    gate = A.f32(183 * KB, 2048).rearrange("p (h q n) -> p h q n", h=8, q=8, n=32)
    mtmp = v3(A.f32(191 * KB, 32), 2, 16)
    zT = v3(A.bf16(44 * KB, 8192), 8, 1024)
    QT = v3(A.bf16(60 * KB, 8192), 8, 1024)

    S.barrier()
    load_w_in(0, 0, 'W0')
    load_w_in(1, 512, 'W1')
    load_w_in(2, 1024, 'W2')
    load_w_in(3, 1536, 'W3')
    S.dma('pool', wpool, w_pool.rearrange("g (i p) o -> p g i o", p=128), 'wpool', writes=['wpool'])

    pi = 0
    for c in range(8):
        for th in range(2):
            pb = pi % 4
            pi += 1
            for dc in range(16):
                S.op('pe', lambda e: e.matmul(B[pb], lhsT=W[c // 4][:, dc, (c % 4) * 128:(c % 4 + 1) * 128],
                                              rhs=hT[th][:, dc, :], start=(dc == 0), stop=(dc == 15)),
                     reads=[f'W{c // 4}'] + hk[th], writes=[f'B{pb}'], inc=(dc == 15))
            S.op('act', lambda e: e.activation(out=uT[:, c, 16 + th * 512:16 + (th + 1) * 512], in_=B[pb], func=AF.Copy),
                 reads=[f'B{pb}'], writes=[f'uT{c}'])
        pb = pi % 4
        pi += 1
        for dc in range(16):
            S.op('pe', lambda e: e.matmul(B[pb][:, 0:16], lhsT=W[c // 4][:, dc, (c % 4) * 128:(c % 4 + 1) * 128],
                                          rhs=hTh[:, dc, 112:128], start=(dc == 0), stop=(dc == 15)),
                 reads=[f'W{c // 4}', 'hTh'], writes=[f'B{pb}'], inc=(dc == 15))
        S.op('act', lambda e: e.activation(out=uT[:, c, 0:16], in_=B[pb][:, 0:16], func=AF.Copy),
             reads=[f'B{pb}'], writes=[f'uT{c}'])
    dbg("uT", uT, [128, 8, 1040], F32, [f'uT{c}' for c in range(8)])
    for gi in range(4):
        src = uT[:, 2 * gi:2 * gi + 2, :]
        cur, ckey = src, None
        ukeys = [f'uT{2 * gi}', f'uT{2 * gi + 1}']
        for k in range(gi + 1):
            sh, lo = 2 ** k, 2 ** (k + 1) - 1
            dst, dkey = (tA, 'tA') if k % 2 == 0 else (tB, 'tB')
            S.op('dve', lambda e: e.tensor_tensor(out=dst[:, :, lo:1040], in0=cur[:, :, lo:1040],
                                                  in1=cur[:, :, lo - sh:1040 - sh], op=ALU.add),
                 reads=ukeys + ([ckey] if ckey else []), writes=[dkey])
            cur, ckey = dst, dkey
        w = 2 ** (gi + 1)
        S.op('dve', lambda e: e.scalar_tensor_tensor(out=mixT[:, 2 * gi:2 * gi + 2, :], in0=cur[:, :, 16:1040],
                                                     scalar=1.0 / w, in1=src[:, :, 16:1040], op0=ALU.mult,
                                                     op1=ALU.subtract),
             reads=ukeys + [ckey], writes=[f'mix{gi}'])
        S.op('dve', lambda e: e.tensor_tensor(out=mtmp, in0=cur[:, :, 16:32], in1=invcnt[:, 2 * gi:2 * gi + 2, :],
                                              op=ALU.mult), reads=[ckey, 'invcnt'], writes=['mtmp'])
        S.op('dve', lambda e: e.tensor_tensor(out=mixT[:, 2 * gi:2 * gi + 2, 0:16], in0=mtmp, in1=src[:, :, 16:32],
                                              op=ALU.subtract), reads=['mtmp', f'mix{gi}'] + ukeys, writes=[f'mix{gi}'])
    dbg("mixT", mixT, [128, 8, 1024], BF16, [f'mix{gi}' for gi in range(4)])
    for gi in range(4):
        for oc in range(2):
            for th in range(2):
                pb = pi % 4
                pi += 1
                for ic in range(2):
                    S.op('pe', lambda e: e.matmul(B[pb], lhsT=wpool[:, gi, ic, oc * 128:(oc + 1) * 128],
                                                  rhs=mixT[:, 2 * gi + ic, th * 512:(th + 1) * 512],
                                                  start=(ic == 0), stop=(ic == 1)),
                         reads=['wpool', f'mix{gi}'], writes=[f'B{pb}'], inc=(ic == 1))
                S.op('dve', lambda e: e.tensor_scalar(out=zT[:, 2 * gi + oc, th * 512:(th + 1) * 512], in0=B[pb],
                                                      scalar1=pst[:, 2 * gi + oc:2 * gi + oc + 1], scalar2=None,
                                                      op0=ALU.mult),
                     reads=[f'B{pb}', 'pst'], writes=['zT', 'W0'])
    dbg("zT", zT, [128, 8, 1024], BF16, ['zT'])
    if stop_after <= 1.5:
        return finish(store_toks)

    kall = ['ksum'] + [f'ksum{g}' for g in range(16)]
    S.op('dve', lambda e: e.tensor_copy(out=kshi, in_=ksum), reads=kall, writes=['kshi'])
    S.op('dve', lambda e: e.scalar_tensor_tensor(out=kslo, in0=kshi, scalar=-1.0, in1=ksum, op0=ALU.mult, op1=ALU.add), reads=kall + ['kshi'],
         writes=['kslo'])
    for h in range(8):
        for th in range(2):
            pb = pi % 4
            pi += 1
            qs = (h * 2 + th) % 2
            for dc in range(16):
                S.op('pe', lambda e: e.matmul(B[pb], lhsT=W[2 + h // 4][:, dc, (h % 4) * 128:(h % 4 + 1) * 128],
                                              rhs=hT[th][:, dc, :], start=(dc == 0), stop=(dc == 15)),
                     reads=[f'W{2 + h // 4}'] + hk[th], writes=[f'B{pb}'], inc=(dc == 15))
            S.op('act', lambda e: e.activation(out=QT[:, h, th * 512:(th + 1) * 512], in_=B[pb], func=AF.Copy,
                                               scale=SCALE), reads=[f'B{pb}'], writes=['QT', 'W1', f'Bsync{pb}'])
            if stop_after <= 1.6:
                continue
            S.op('dve', lambda e: e.tensor_copy(out=qhi[qs], in_=B[pb]), reads=[f'B{pb}', f'Bsync{pb}'], writes=[f'qhi{qs}'])
            S.op('dve', lambda e: e.scalar_tensor_tensor(out=qlo[qs], in0=qhi[qs], scalar=-1.0, in1=B[pb], op0=ALU.mult, op1=ALU.add),
                 reads=[f'B{pb}', f'qhi{qs}'], writes=[f'qlo{qs}'])
            if stop_after <= 1.65:
                continue
            for qi in range(4):
                qt = th * 4 + qi
                pg = 4 + qt % 4
                qsl_ = slice(qi * 128, (qi + 1) * 128)
                S.op('pe', lambda e: e.matmul(B[pg][:, 0:32], lhsT=qhi[qs][:, qsl_], rhs=kshi[:, h, :], start=True,
                                              stop=False), reads=[f'qhi{qs}', f'qlo{qs}', 'kshi', 'kslo'],
                     writes=[f'B{pg}'], inc=False)
                S.op('pe', lambda e: e.matmul(B[pg][:, 0:32], lhsT=qhi[qs][:, qsl_], rhs=kslo[:, h, :], start=False,
                                              stop=False), reads=[f'qhi{qs}', f'qlo{qs}', 'kshi', 'kslo'],
                     writes=[f'B{pg}'], inc=False)
                S.op('pe', lambda e: e.matmul(B[pg][:, 0:32], lhsT=qlo[qs][:, qsl_], rhs=kshi[:, h, :], start=False,
                                              stop=True), reads=[f'qhi{qs}', f'qlo{qs}', 'kshi', 'kslo'],
                     writes=[f'B{pg}'])
                S.op('dve', lambda e: e.tensor_tensor(out=gate[:, h, qt, :], in0=B[pg][:, 0:32], in1=padmask,
                                                      op=ALU.add),
                     reads=[f'B{pg}', 'padmask'], writes=['gate'])
    if stop_after > 1.65:
        dbg("gate", gate, [128, 8, 8, 32], F32, ['gate'])
    dbg("QT", QT, [128, 8, 1024], BF16, ['QT'])
    if stop_after <= 1.7:
        return finish(store_toks)
    for h in range(8):
        for qt in range(8):
            j = qt // 2
            gv = gate[:, h, qt, :]
            if 28 + j < 32:
                S.op('dve', lambda e: e.memset(gate[:, h, qt, 28 + j:32], NEG), reads=['gate'], writes=['gate'])
            S.op('dve', lambda e: e.max(out=m8, in_=gv), reads=['gate'], writes=['m8'])
            S.op('dve', lambda e: e.tensor_scalar(out=selt, in0=gv, scalar1=m8[:, 2:3], scalar2=None, op0=ALU.is_ge),
                 reads=['gate', 'm8'], writes=['selt'])
            S.op('dve', lambda e: e.scalar_tensor_tensor(out=gv, in0=gv, scalar=-1.0e29, in1=selt, op0=ALU.is_gt,
                                                         op1=ALU.mult),
                 reads=['gate', 'selt'], writes=['gate'])
    sel = gate
    dbg("sel", sel, [128, 8, 8, 32], F32, ['gate'])
    if stop_after <= 2:
        return finish(store_toks)

    KTb = [A.bf16(76 * KB + i * 16 * KB, 8192) for i in range(2)]
    Vh = [v3(A.bf16(108 * KB + i * 16640, 64 * 129 + 64), 64, 130)[:, :, 0:129] for i in range(2)]
    PT = [A.bf16(142 * KB + i * KB, 512) for i in range(3)]
    Oacc = [v3(A.f32(145 * KB + i * 1280, 258), 2, 129) for i in range(2)]
    atok = [A.bf16(148 * KB + i * 256, 128) for i in range(2)]
    attnT = v3(A.bf16(149 * KB, 8192), 8, 1024)
    Ef = [v3(A.f32(165 * KB + i * KB, 224), 8, 28) for i in range(2)]
    Wn = [v3(A.f32(167 * KB + i * 256, 64), 8, 8) for i in range(2)]
    S.barrier()
    for t in store_toks:
        S.wait_tok('sp', t)
    si = oi = pti = 0
    for h in range(8):
        hb = h % 2
        sl = SLOPES[h]
        S.dma('sp', KTb[hb], kt_d[h], f'KT{hb}', writes=[f'KT{hb}'])
        for c0 in range(0, 64, 16):
            S.dma('sp', Vh[hb][:, c0:c0 + 16, 0:128],
                  vs_d[c0 * 128:(c0 + 16) * 128, h * 128:(h + 1) * 128].rearrange("(c p) d -> p c d", p=128),
                  f'V{hb}', writes=[f'V{hb}'])
        S.op('dve', lambda e: e.memset(Vh[hb][:, :, 128:129], 1.0), reads=[f'V{hb}'], writes=[f'V{hb}'])
        vev = Vh[hb][:, 0:56, :].rearrange("p (b two) d -> p b two d", two=2)[:, :, 0, :]
        S.op('dve', lambda e: e.tensor_scalar(out=vev, in0=vev, scalar1=float(np.exp(-sl * 128.0)), scalar2=None,
                                              op0=ALU.mult), reads=[f'V{hb}'], writes=[f'V{hb}'])
        S.op('act', lambda e: e.activation(out=Ef[hb], in_=Dall, func=AF.Exp, scale=-sl),
             reads=['Dall'], writes=[f'Ef{hb}'])
        S.op('dve', lambda e: e.tensor_tensor(out=Ef[hb][:, :, :], in0=Ef[hb][:, :, :], in1=sel[:, h, :, 0:28],
                                              op=ALU.mult), reads=[f'Ef{hb}', 'gate'], writes=[f'Ef{hb}'])
        S.op('act', lambda e: e.activation(out=NWr[hb], in_=Dnr, func=AF.Exp, scale=-sl),
             reads=['Dnr'], writes=[f'NWr{hb}'])
        for qt in range(8):
            for b in range(qt // 2):
                S.op('dve', lambda e: e.tensor_scalar(out=Wn[hb][:, qt, 2 * b:2 * b + 2],
                                                      in0=NWr[hb][:, 7 - qt + 2 * b:7 - qt + 2 * b + 2],
                                                      scalar1=sel[:, h, qt, 28 + b:29 + b], scalar2=None, op0=ALU.mult),
                     reads=[f'NWr{hb}', 'gate'], writes=[f'Wn{hb}'])
        for j in range(4):
            ob = (h * 4 + j) % 2
            qsl = QT[:, h, j * 256:(j + 1) * 256]
            S.op('dve', lambda e: e.memset(Oacc[ob], 0.0), writes=[f'Oacc{ob}'])
            items = [('far', n) for n in range(28)] + [('near', c) for c in range(2 * j + 2)]

            def stage1(it):
                nonlocal si, pti
                kind, idx = it
                ps = si % 3
                si += 1
                pt = pti % 3
                pti += 1
                if kind == 'far':
                    n = idx
                    for kc in range(2):
                        S.op('pe', lambda e: e.matmul(B[ps][:, kc * 256:(kc + 1) * 256],
                                                      lhsT=KTb[hb][:, n * 256 + kc * 128:n * 256 + (kc + 1) * 128],
                                                      rhs=qsl, start=True, stop=True),
                             reads=[f'KT{hb}', 'QT'], writes=[f'B{ps}'], inc=(kc == 1))
                    S.op('act', lambda e: e.activation(out=PT[pt], in_=B[ps], func=AF.Exp, bias=kbias[:, h:h + 1]),
                         reads=[f'B{ps}', 'kbias'], writes=[f'PT{pt}'])
                else:
                    c = idx
                    S.op('pe', lambda e: e.matmul(B[ps][:, 0:256], lhsT=KTb[hb][:, (56 + c) * 128:(57 + c) * 128],
                                                  rhs=qsl, start=True, stop=True),
                         reads=[f'KT{hb}', 'QT'], writes=[f'B{ps}'])
                    S.op('act', lambda e: e.activation(out=PT[pt][:, 0:256], in_=B[ps][:, 0:256], func=AF.Exp,
                                                       bias=kbias[:, h:h + 1]),
                         reads=[f'B{ps}', 'kbias'], writes=[f'PT{pt}'])
                return pt

            def stage2(it, pt):
                nonlocal oi
                kind, idx = it
                if kind == 'far':
                    n = idx
                    for qc in range(2):
                        po = 3 + oi % 4
                        oi += 1
                        for kc in range(2):
                            S.op('pe', lambda e: e.matmul(B[po][:, 0:129],
                                                          lhsT=PT[pt][:, kc * 256 + qc * 128:kc * 256 + (qc + 1) * 128],
                                                          rhs=Vh[hb][:, 2 * n + kc, :], start=(kc == 0), stop=(kc == 1)),
                                 reads=[f'PT{pt}', f'V{hb}'], writes=[f'B{po}'], inc=(kc == 1))
                        S.op('dve', lambda e: e.scalar_tensor_tensor(out=Oacc[ob][:, qc, :], in0=B[po][:, 0:129],
                                                                     scalar=Ef[hb][:, 2 * j + qc, n:n + 1],
                                                                     in1=Oacc[ob][:, qc, :], op0=ALU.mult, op1=ALU.add),
                             reads=[f'B{po}', f'Ef{hb}', f'Oacc{ob}'], writes=[f'Oacc{ob}'])
                else:
                    c = idx
                    for qc in range(2):
                        qt = 2 * j + qc
                        if c > qt:
                            continue
                        if c == qt:
                            S.op('dve', lambda e: e.tensor_tensor(out=PT[pt][:, qc * 128:(qc + 1) * 128],
                                                                  in0=PT[pt][:, qc * 128:(qc + 1) * 128], in1=tri,
                                                                  op=ALU.mult),
                                 reads=[f'PT{pt}', 'tri'], writes=[f'PT{pt}'])
                        po = 3 + oi % 4
                        oi += 1
                        S.op('pe', lambda e: e.matmul(B[po][:, 0:129], lhsT=PT[pt][:, qc * 128:(qc + 1) * 128],
                                                      rhs=Vh[hb][:, 56 + c, :], start=True, stop=True),
                             reads=[f'PT{pt}', f'V{hb}'], writes=[f'B{po}'])
                        if c // 2 < qt // 2:
                            wap, wkey = Wn[hb][:, qt, c:c + 1], f'Wn{hb}'
                        else:
                            wap, wkey = NWr[hb][:, 7 - qt + c:8 - qt + c], f'NWr{hb}'
                        S.op('dve', lambda e: e.scalar_tensor_tensor(out=Oacc[ob][:, qc, :], in0=B[po][:, 0:129],
                                                                     scalar=wap, in1=Oacc[ob][:, qc, :], op0=ALU.mult,
                                                                     op1=ALU.add),
                             reads=[f'B{po}', wkey, f'Oacc{ob}'], writes=[f'Oacc{ob}'])

            prev = None
            for it in items:
                ptc = stage1(it)
                if prev is not None:
                    stage2(*prev)
                prev = (it, ptc)
            stage2(*prev)
            for qc in range(2):
                qt = 2 * j + qc
                S.op('dve', lambda e: e.reciprocal(out=rden[:, qc:qc + 1], in_=Oacc[ob][:, qc, 128:129]),
                     reads=[f'Oacc{ob}'], writes=[f'rden{qc}'])
                S.op('dve', lambda e: e.tensor_scalar(out=atok[qc], in0=Oacc[ob][:, qc, 0:128],
                                                      scalar1=rden[:, qc:qc + 1], scalar2=None, op0=ALU.mult),
                     reads=[f'Oacc{ob}', f'rden{qc}'], writes=[f'atok{qc}'])
                S.op('pe', lambda e: e.transpose(Bb[7][:, qc * 128:(qc + 1) * 128], atok[qc], ident_b),
                     reads=[f'atok{qc}', 'ident_b'], writes=['B7'])
                S.op('act', lambda e: e.activation(out=attnT[:, h, qt * 128:(qt + 1) * 128],
                                                   in_=Bb[7][:, qc * 128:(qc + 1) * 128], func=AF.Copy),
                     reads=['B7'], writes=['attnT'])
    dbg("attnT", attnT, [128, 8, 1024], BF16, ['attnT'])
    if stop_after <= 3:
        return finish()

    mergedT = v3(A.bf16(76 * KB, 16384), 16, 1024)
    U4 = [108 * KB, 165 * KB]
    wgp = [v3(A.bf16(U4[i], 4096), 16, 256) for i in range(2)]
    wga = [v3(A.bf16(U4[i] + 8 * KB, 4096), 16, 256) for i in range(2)]
    wbp = [v3(A.bf16(U4[i] + 16 * KB, 2048), 8, 256) for i in range(2)]
    wba = [v3(A.bf16(U4[i] + 20 * KB, 2048), 8, 256) for i in range(2)]
    sg1 = [A.f32(132 * KB + i * 2 * KB, 512) for i in range(2)]
    sg2 = [A.f32(136 * KB + i * 2 * KB, 512) for i in range(2)]
    S.barrier()

    def load_u4(u):
        s = u % 2
        extra = []
        S.dma('pool', wgp[s], w_in[:, 4096 + u * 256:4096 + (u + 1) * 256].rearrange("(c p) f -> p c f", p=128),
              f'wgp{s}', writes=[f'wgp{s}'] + extra)
        S.dma('pool', wga[s], w_in[:, 6144 + u * 256:6144 + (u + 1) * 256].rearrange("(c p) f -> p c f", p=128),
              f'wga{s}', writes=[f'wga{s}'] + extra)
        S.dma('pool', wbp[s], w_bp[:, u * 256:(u + 1) * 256].rearrange("(c p) f -> p c f", p=128),
              f'wbp{s}', writes=[f'wbp{s}'] + extra)
        S.dma('pool', wba[s], w_ba[:, u * 256:(u + 1) * 256].rearrange("(c p) f -> p c f", p=128),
              f'wba{s}', writes=[f'wba{s}'] + extra)

    load_u4(0)
    load_u4(1)
    ui = 0
    for u in range(8):
        s = u % 2
        for fl in range(2):
            fcx = u * 2 + fl
            fs = slice(fl * 128, (fl + 1) * 128)
            for th in range(2):
                par = ui % 2
                ui += 1
                ts_ = slice(th * 512, (th + 1) * 512)
                for dc in range(16):
                    S.op('pe', lambda e: e.matmul(B[0 + par], lhsT=wgp[s][:, dc, fs], rhs=hT[th][:, dc, :],
                                                  start=(dc == 0), stop=(dc == 15)),
                         reads=[f'wgp{s}'] + hk[th], writes=[f'B{par}'], inc=(dc == 15))
                for dc in range(16):
                    S.op('pe', lambda e: e.matmul(B[2 + par], lhsT=wga[s][:, dc, fs], rhs=hT[th][:, dc, :],
                                                  start=(dc == 0), stop=(dc == 15)),
                         reads=[f'wga{s}'] + hk[th], writes=[f'B{2 + par}'], inc=(dc == 15))
                for c in range(8):
                    S.op('pe', lambda e: e.matmul(B[4 + par], lhsT=wbp[s][:, c, fs], rhs=zT[:, c, ts_],
                                                  start=(c == 0), stop=(c == 7)),
                         reads=[f'wbp{s}', 'zT'], writes=[f'B{4 + par}'], inc=(c == 7))
                for c in range(8):
                    S.op('pe', lambda e: e.matmul(B[6 + par], lhsT=wba[s][:, c, fs], rhs=attnT[:, c, ts_],
                                                  start=(c == 0), stop=(c == 7)),
                         reads=[f'wba{s}', 'attnT'], writes=[f'B{6 + par}'], inc=(c == 7))
                S.op('act', lambda e: e.activation(out=sg1[par], in_=B[0 + par], func=AF.Sigmoid),
                     reads=[f'B{par}'], writes=[f'sg1{par}'])
                S.op('act', lambda e: e.activation(out=sg2[par], in_=B[2 + par], func=AF.Sigmoid),
                     reads=[f'B{2 + par}'], writes=[f'sg2{par}'])
                S.op('dve', lambda e: e.tensor_tensor(out=sg1[par], in0=sg1[par], in1=B[4 + par], op=ALU.mult),
                     reads=[f'sg1{par}', f'B{4 + par}'], writes=[f'sg1{par}'])
                S.op('dve', lambda e: e.tensor_tensor(out=sg2[par], in0=sg2[par], in1=B[6 + par], op=ALU.mult),
                     reads=[f'sg2{par}', f'B{6 + par}'], writes=[f'sg2{par}'])
                S.op('dve', lambda e: e.tensor_tensor(out=mergedT[:, fcx, ts_], in0=sg1[par], in1=sg2[par], op=ALU.add),
                     reads=[f'sg1{par}', f'sg2{par}'], writes=['mergedT'])
        if u + 2 < 8:
            load_u4(u + 2)
    dbg("mergedT", mergedT, [128, 16, 1024], BF16, ['mergedT'])

    x1 = v3(A.f32(8 * KB, 8 * 2048), 8, 2048)
    wo = [v3(A.bf16(108 * KB + i * 16 * KB, 8192), 16, 512) for i in range(2)]
    S.barrier()
    for tt in range(8):
        S.dma('sp', x1[:, tt, :], xc[7168 + tt * 128:7168 + (tt + 1) * 128, :], f'x1_{tt}',
              writes=[f'x1_{tt}'])
    def load_wo(cb):
        s = cb % 2
        S.dma('pool', wo[s], w_out[:, cb * 512:(cb + 1) * 512].rearrange("(c p) f -> p c f", p=128), f'wo{s}',
              writes=[f'wo{s}'])

    load_wo(0)
    load_wo(1)
    for cb in range(4):
        s = cb % 2
        for tt in range(8):
            pb = tt % 4
            for fc in range(16):
                S.op('pe', lambda e: e.matmul(B[pb], lhsT=mergedT[:, fc, tt * 128:(tt + 1) * 128], rhs=wo[s][:, fc, :],
                                              start=(fc == 0), stop=(fc == 15)),
                     reads=['mergedT', f'wo{s}'], writes=[f'B{pb}'], inc=(fc == 15))
            S.op('dve', lambda e: e.tensor_tensor(out=x1[:, tt, cb * 512:(cb + 1) * 512], in0=B[pb],
                                                  in1=x1[:, tt, cb * 512:(cb + 1) * 512], op=ALU.add),
                 reads=[f'B{pb}', f'x1_{tt}'], writes=[f'x1_{tt}'])
        if cb + 2 < 4:
            load_wo(cb + 2)
    x1k = [f'x1_{tt}' for tt in range(8)]
    dbg("x1", x1, [128, 8, 2048], F32, x1k)
    if stop_after <= 4:
        return finish()

    h2tok = v3(A.bf16(72 * KB, 8 * 2048), 8, 2048)
    Pb = [v3(A.bf16(104 * KB + i * 4 * KB, 2048), 8, 256) for i in range(2)]
    xgT = v3(A.bf16(112 * KB, 4096), 16, 256)
    NR = 7
    ring = [A.bf16(120 * KB + i * 8 * KB, 4096) for i in range(NR)]
    PTe = v3(A.bf16(176 * KB, 2048), 2, 1024)
    sa = [A.f32(180 * KB + i * KB, 256) for i in range(2)]
    h2f = A.f32(184 * KB, 2048)
    ybuf = v3(A.bf16(184 * KB, 4096), 2, 2048)
    h2lo = A.bf16(104 * KB, 2048)
    h2Thi = v3(A.bf16(108 * KB, 2048), 16, 128)
    h2Tlo = v3(A.bf16(112 * KB, 2048), 16, 128)
    gbc = A.f32(194 * KB, 2048)
    hidT = v3(A.bf16(202 * KB, 2048), 8, 256)
    o = 1792
    wr = v3(cf32(320), 16, 20)
    wrhi = v3(cbf(320), 16, 20)
    wrlo = v3(cbf(320), 16, 20)
    assert o <= 4420, o
    o = 192 * KB
    comb = v3(cf32(128), 8, 16)
    lg = cf32(20)
    goh = cf32(4)
    esel = cf32(4)
    esel2 = cf32(4)
    oh1 = cf32(4)
    oh2 = cf32(4)
    ce = cf32(4)
    sc = cf32(16)
    ge = cf32(4)
    mb = v3(cbf(128), 8, 16)
    mf = v3(cf32(128), 8, 16)
    rank = v3(cf32(128), 8, 16)
    assert o <= 194 * KB, o
    o = 182 * KB
    rankJ = v3(cf32(128), 8, 16)
    cntf = cf32(16)
    cnti = A.t[:, o // 4: o // 4 + 16].bitcast(I32)
    o += 64
    iotaS = A.f32(183 * KB, 256)
    assert o <= 183 * KB, o
    S.barrier()
    S.dma('sp', wr, v3(wr_d, 16, 20), 'wr', writes=['wr'])
    S.op('dve', lambda e: e.tensor_copy(out=wrhi, in_=wr), reads=['wr'], writes=['wrhi'])
    S.op('dve', lambda e: e.scalar_tensor_tensor(out=wrlo, in0=wrhi, scalar=-1.0, in1=wr, op0=ALU.mult, op1=ALU.add),
         reads=['wr', 'wrhi'], writes=['wrlo'])
    S.dma('sp', gbc, g2_d, 'gbc', writes=['gbc'])
    S.op('pool', lambda e: e.iota(itmp[:, 0:224], pattern=[[1, 224]], base=0, channel_multiplier=0), writes=['itmp'])
    S.op('dve', lambda e: e.tensor_copy(out=iotaS[:, 0:224], in_=itmp[:, 0:224]), reads=['itmp'], writes=['iotaS'])
    S.op('pool', lambda e: e.iota(itmp[:, 0:32], pattern=[[1, 32]], base=224, channel_multiplier=0),
         reads=['iotaS'], writes=['itmp'])
    S.op('dve', lambda e: e.tensor_copy(out=iotaS[:, 224:256], in_=itmp[:, 0:32]), reads=['itmp', 'iotaS'],
         writes=['iotaS'])
    units = []
    for ex in range(16):
        for q in range(4):
            units.append(('g', ex, q))
            units.append(('u', ex, q))
        for q in range(4):
            units.append(('d', ex, q))
    NU = len(units)

    def load_unit(k):
        kind, ex, q = units[k]
        s = k % NR
        if kind == 'd':
            dst = v3(ring[s], 2, 2048)
            src = w_down[ex, q * 256:(q + 1) * 256, :].rearrange("(c p) d -> p c d", p=128)
        else:
            wsrc = w_gate if kind == 'g' else w_up
            dst = v3(ring[s], 16, 256)
            src = wsrc[ex, :, q * 256:(q + 1) * 256].rearrange("(c p) f -> p c f", p=128)
        S.dma('pool', dst, src, f'ring{s}', writes=[f'ring{s}'])

    PRE = NR - 1
    for k in range(PRE):
        load_unit(k)
    nextload = PRE
    S.op('dve', lambda e: e.memset(ssq, 0.0), writes=['ssq2'])
    for tt in range(8):
        S.op('act', lambda e: e.activation(out=h2f, in_=x1[:, tt, :], func=AF.Square, accum_out=ssq[:, tt:tt + 1]),
             reads=[f'x1_{tt}', 'ssq2'], writes=['h2f', f'ssqb{tt}'])
        S.op('act', lambda e: e.activation(out=rstd[:, tt:tt + 1], in_=ssq[:, tt:tt + 1], func=AF.Sqrt,
                                           scale=1.0 / 2048, bias=epst), reads=[f'ssqb{tt}', 'epst'], writes=[f'rstdb{tt}'])
        S.op('dve', lambda e: e.reciprocal(out=rstd[:, tt:tt + 1], in_=rstd[:, tt:tt + 1]),
             reads=[f'rstdb{tt}'], writes=[f'rstdb{tt}'])
        S.op('dve', lambda e: e.scalar_tensor_tensor(out=h2f, in0=x1[:, tt, :], scalar=rstd[:, tt:tt + 1], in1=gbc,
                                                     op0=ALU.mult, op1=ALU.mult),
             reads=[f'x1_{tt}', f'rstdb{tt}', 'gbc', 'h2f'], writes=['h2f'])
        h2hi = h2tok[:, tt, :]
        S.op('dve', lambda e: e.tensor_copy(out=h2hi, in_=h2f), reads=['h2f'], writes=[f'h2tok{tt}'])
        S.op('dve', lambda e: e.scalar_tensor_tensor(out=h2lo, in0=h2hi, scalar=-1.0, in1=h2f, op0=ALU.mult,
                                                     op1=ALU.add), reads=['h2f', f'h2tok{tt}'], writes=['h2lo'])
        for half in range(2):
            for k8 in range(8):
                dc = half * 8 + k8
                S.op('pe', lambda e: e.transpose(Bb[half][:, k8 * 128:(k8 + 1) * 128], h2hi[:, dc * 128:(dc + 1) * 128],
                                                 ident_b), reads=[f'h2tok{tt}', 'ident_b'], writes=[f'B{half}'],
                     inc=(k8 == 7))
            S.op('act', lambda e: e.activation(out=h2Thi[:, half * 8:(half + 1) * 8, :], in_=v3(Bb[half], 8, 128),
                                               func=AF.Copy), reads=[f'B{half}'], writes=['h2Thi'])
        for half in range(2):
            for k8 in range(8):
                dc = half * 8 + k8
                S.op('pe', lambda e: e.transpose(Bb[2 + half][:, k8 * 128:(k8 + 1) * 128],
                                                 h2lo[:, dc * 128:(dc + 1) * 128], ident_b),
                     reads=['h2lo', 'ident_b'], writes=[f'B{2 + half}'], inc=(k8 == 7))
            S.op('act', lambda e: e.activation(out=h2Tlo[:, half * 8:(half + 1) * 8, :], in_=v3(Bb[2 + half], 8, 128),
                                               func=AF.Copy), reads=[f'B{2 + half}'], writes=['h2Tlo'])
        rk = ['h2Thi', 'h2Tlo', 'wrhi', 'wrlo']
        for dc in range(16):
            S.op('pe', lambda e: e.matmul(B[6][:, 0:20], lhsT=h2Thi[:, dc, :], rhs=wrhi[:, dc, :], start=(dc == 0),
                                          stop=False), reads=rk, writes=['B6'], inc=False)
            S.op('pe', lambda e: e.matmul(B[6][:, 0:20], lhsT=h2Thi[:, dc, :], rhs=wrlo[:, dc, :], start=False,
                                          stop=False), reads=rk, writes=['B6'], inc=False)
            S.op('pe', lambda e: e.matmul(B[6][:, 0:20], lhsT=h2Tlo[:, dc, :], rhs=wrhi[:, dc, :], start=False,
                                          stop=(dc == 15)), reads=rk, writes=['B6'], inc=(dc == 15))
        S.op('dve', lambda e: e.tensor_tensor(out=lg, in0=B[6][:, 0:20], in1=br, op=ALU.add),
             reads=['B6', 'br'], writes=['lg'])
        S.op('dve', lambda e: e.tensor_reduce(out=sc[:, 0:1], in_=lg[:, 0:4], axis=AX.X, op=ALU.max),
             reads=['lg'], writes=['sc0'])
        S.op('dve', lambda e: e.tensor_scalar(out=goh, in0=lg[:, 0:4], scalar1=sc[:, 0:1], scalar2=None, op0=ALU.is_ge),
             reads=['lg', 'sc0'], writes=['goh'])
        S.op('dve', lambda e: e.tensor_scalar(out=sc[:, 1:2], in0=sc[:, 0:1], scalar1=-1.0, scalar2=None, op0=ALU.mult),
             reads=['sc0'], writes=['sc1'])
        S.op('dve', lambda e: e.memset(sc[:, 2:3], 0.0), writes=['sc2'])
        S.op('act', lambda e: e.activation(out=ge, in_=lg[:, 0:4], func=AF.Exp, bias=sc[:, 1:2], accum_out=sc[:, 2:3]),
             reads=['lg', 'sc1', 'sc2'], writes=['ge', 'sc2'])
        S.op('dve', lambda e: e.reciprocal(out=sc[:, 3:4], in_=sc[:, 2:3]), reads=['sc2'], writes=['sc3'])
        S.op('dve', lambda e: e.tensor_scalar(out=esel, in0=lg[:, 4:8], scalar1=goh[:, 0:1], scalar2=None, op0=ALU.mult),
             reads=['lg', 'goh'], writes=['esel'])
        for g in range(1, 4):
            S.op('dve', lambda e: e.scalar_tensor_tensor(out=esel, in0=lg[:, 4 + 4 * g:8 + 4 * g], scalar=goh[:, g:g + 1],
                                                         in1=esel, op0=ALU.mult, op1=ALU.add),
                 reads=['lg', 'goh', 'esel'], writes=['esel'])
        S.op('dve', lambda e: e.tensor_reduce(out=sc[:, 4:5], in_=esel, axis=AX.X, op=ALU.max),
             reads=['esel'], writes=['sc4'])
        S.op('dve', lambda e: e.tensor_scalar(out=oh1, in0=esel, scalar1=sc[:, 4:5], scalar2=None, op0=ALU.is_ge),
             reads=['esel', 'sc4'], writes=['oh1'])
        S.op('dve', lambda e: e.scalar_tensor_tensor(out=esel2, in0=oh1, scalar=NEG, in1=esel, op0=ALU.mult, op1=ALU.add),
             reads=['oh1', 'esel'], writes=['esel2'])
        S.op('dve', lambda e: e.tensor_reduce(out=sc[:, 5:6], in_=esel2, axis=AX.X, op=ALU.max),
             reads=['esel2'], writes=['sc5'])
        S.op('dve', lambda e: e.tensor_scalar(out=oh2, in0=esel2, scalar1=sc[:, 5:6], scalar2=None, op0=ALU.is_ge),
             reads=['esel2', 'sc5'], writes=['oh2'])
        S.op('dve', lambda e: e.tensor_tensor(out=sc[:, 6:7], in0=sc[:, 5:6], in1=sc[:, 4:5], op=ALU.subtract),
             reads=['sc4', 'sc5'], writes=['sc6'])
        S.op('act', lambda e: e.activation(out=sc[:, 7:8], in_=sc[:, 6:7], func=AF.Exp), reads=['sc6'], writes=['sc7'])
        S.op('dve', lambda e: e.tensor_scalar(out=sc[:, 8:9], in0=sc[:, 7:8], scalar1=1.0, scalar2=None, op0=ALU.add),
             reads=['sc7'], writes=['sc8'])
        S.op('dve', lambda e: e.reciprocal(out=sc[:, 9:10], in_=sc[:, 8:9]), reads=['sc8'], writes=['sc9'])
        S.op('dve', lambda e: e.tensor_tensor(out=sc[:, 10:11], in0=sc[:, 7:8], in1=sc[:, 9:10], op=ALU.mult),
             reads=['sc7', 'sc9'], writes=['sc10'])
        S.op('dve', lambda e: e.tensor_tensor(out=sc[:, 11:12], in0=sc[:, 9:10], in1=sc[:, 3:4], op=ALU.mult),
             reads=['sc9', 'sc3'], writes=['sc11'])
        S.op('dve', lambda e: e.tensor_tensor(out=sc[:, 12:13], in0=sc[:, 10:11], in1=sc[:, 3:4], op=ALU.mult),
             reads=['sc10', 'sc3'], writes=['sc12'])
        S.op('dve', lambda e: e.tensor_scalar(out=ce, in0=oh1, scalar1=sc[:, 11:12], scalar2=None, op0=ALU.mult),
             reads=['oh1', 'sc11'], writes=['ce'])
        S.op('dve', lambda e: e.scalar_tensor_tensor(out=ce, in0=oh2, scalar=sc[:, 12:13], in1=ce, op0=ALU.mult,
                                                     op1=ALU.add), reads=['oh2', 'sc12', 'ce'], writes=['ce'])
        for g in range(4):
            S.op('dve', lambda e: e.tensor_scalar(out=comb[:, tt, 4 * g:4 * g + 4], in0=ce, scalar1=goh[:, g:g + 1],
                                                  scalar2=None, op0=ALU.mult), reads=['ce', 'goh'], writes=['comb'])
    dbg("comb", comb, [128, 8, 16], F32, ['comb'])
    S.op('dve', lambda e: e.tensor_scalar(out=mf, in0=comb, scalar1=0.0, scalar2=None, op0=ALU.is_gt),
         reads=['comb'], writes=['mf'])
    S.op('dve', lambda e: e.tensor_copy(out=mb, in_=mf), reads=['mf'], writes=['mb'])
    for tt in range(8):
        for t2 in range(tt):
            S.op('pe', lambda e: e.matmul(B[0][:, tt * 16:(tt + 1) * 16], lhsT=ones_sq, rhs=mb[:, t2, :],
                                          start=(t2 == 0), stop=False), reads=['mb', 'ones_sq'], writes=['B0'], inc=False)
        S.op('pe', lambda e: e.matmul(B[0][:, tt * 16:(tt + 1) * 16], lhsT=tri, rhs=mb[:, tt, :], start=(tt == 0),
                                      stop=True), reads=['mb', 'tri', 'ones_sq'], writes=['B0'])
    for tt in range(8):
        S.op('pe', lambda e: e.matmul(B[1][:, 0:16], lhsT=ones_sq, rhs=mb[:, tt, :], start=(tt == 0), stop=(tt == 7)),
             reads=['mb', 'ones_sq'], writes=['B1'], inc=(tt == 7))
    S.op('dve', lambda e: e.tensor_tensor(out=rank, in0=v3(B[0][:, 0:128], 8, 16), in1=mf, op=ALU.mult),
         reads=['B0', 'mf'], writes=['rank'])
    S.op('dve', lambda e: e.tensor_scalar(out=rank, in0=rank, scalar1=-1.0, scalar2=None, op0=ALU.add),
         reads=['rank'], writes=['rank'])
    S.op('dve', lambda e: e.tensor_scalar(out=cntf, in0=B[1][:, 0:16], scalar1=-1.0, scalar2=1024.0, op0=ALU.mult,
                                          op1=ALU.add), reads=['B1'], writes=['cntf'])
    S.op('dve', lambda e: e.tensor_copy(out=cnti, in_=cntf), reads=['cntf'], writes=['cnti'])
    dbg("rank", rank, [128, 8, 16], F32, ['rank'])
    dbg("cntf", cntf, [128, 16], F32, ['cntf'])

    ctr = {'k': 0, 'g': 0, 'au': 0, 'dn': 0, 'sc': 0, 'pb': 0}

    def emit_block(ex, J, slots, after_q=None):
        gs_, us_, ds_ = slots
        pb = ctr['pb'] % 2
        ctr['pb'] += 1
        P = Pb[pb]
        if J == 0:
            rk_ap, rkey = rank, 'rank'
        else:
            S.op('dve', lambda e: e.tensor_scalar(out=rankJ[:, :, ex:ex + 1], in0=rank[:, :, ex:ex + 1],
                                                  scalar1=-256.0 * J, scalar2=None, op0=ALU.add),
                 reads=['rank'], writes=['rankJ'])
            rk_ap, rkey = rankJ, 'rankJ'
        for tt in range(8):
            S.op('dve', lambda e: e.tensor_scalar(out=P[:, tt, :], in0=iotaS, scalar1=rk_ap[:, tt, ex:ex + 1],
                                                  scalar2=None, op0=ALU.is_equal),
                 reads=['iotaS', rkey], writes=[f'P{pb}', 'h2lo', 'h2Thi'])
        for dp in range(8):
            gb = ctr['g'] % 2
            ctr['g'] += 1
            for dl in range(2):
                dc = dp * 2 + dl
                for tt in range(8):
                    S.op('pe', lambda e: e.matmul(B[gb][:, dl * 256:(dl + 1) * 256],
                                                  lhsT=h2tok[:, tt, dc * 128:(dc + 1) * 128], rhs=P[:, tt, :],
                                                  start=(tt == 0), stop=(tt == 7)),
                         reads=[f'P{pb}'] + [f'h2tok{t_}' for t_ in range(8)], writes=[f'B{gb}'],
                         inc=(dl == 1 and tt == 7))
            S.op('act', lambda e: e.activation(out=xgT[:, dp * 2:dp * 2 + 2, :], in_=v3(B[gb], 2, 256), func=AF.Copy),
                 reads=[f'B{gb}'], writes=['xgT', 'h2Thi', 'h2Tlo', 'h2lo'])
        for st in range(2):
            for tt in range(8):
                S.op('pe', lambda e: e.transpose(Bb[6][:, tt * 128:(tt + 1) * 128], P[:, tt, st * 128:(st + 1) * 128],
                                                 ident_b), reads=[f'P{pb}', 'ident_b'], writes=['B6'], inc=(tt == 7))
            S.op('act', lambda e: e.activation(out=PTe[:, st, :], in_=Bb[6], func=AF.Copy), reads=['B6'],
                 writes=['PTe'])
        for q in range(4):
            G = v3(ring[gs_[q]], 16, 256)
            U = v3(ring[us_[q]], 16, 256)
            for fl in range(2):
                fc = 2 * q + fl
                fs = slice(fl * 128, (fl + 1) * 128)
                ab = 2 + ctr['au'] % 2
                ctr['au'] += 1
                for dc in range(16):
                    S.op('pe', lambda e: e.matmul(B[ab][:, 0:256], lhsT=G[:, dc, fs], rhs=xgT[:, dc, :],
                                                  start=(dc == 0), stop=(dc == 15)),
                         reads=[f'ring{gs_[q]}', 'xgT'], writes=[f'B{ab}'], inc=False)
                for dc in range(16):
                    S.op('pe', lambda e: e.matmul(B[ab][:, 256:512], lhsT=U[:, dc, fs], rhs=xgT[:, dc, :],
                                                  start=(dc == 0), stop=(dc == 15)),
                         reads=[f'ring{us_[q]}', 'xgT'], writes=[f'B{ab}'], inc=(dc == 15))
                sb_ = ab - 2
                S.op('act', lambda e: e.activation(out=sa[sb_], in_=B[ab][:, 0:256], func=AF.Silu),
                     reads=[f'B{ab}'], writes=[f'sa{sb_}'])
                S.op('dve', lambda e: e.tensor_tensor(out=hidT[:, fc, :], in0=sa[sb_], in1=B[ab][:, 256:512], op=ALU.mult),
                     reads=[f'sa{sb_}', f'B{ab}'], writes=['hidT'])
            if after_q is not None:
                after_q(q)
        for st in range(2):
            for cb in range(4):
                db = 4 + ctr['dn'] % 2
                ctr['dn'] += 1
                for fc in range(8):
                    D = v3(ring[ds_[fc // 2]], 2, 2048)
                    S.op('pe', lambda e: e.matmul(B[db], lhsT=hidT[:, fc, st * 128:(st + 1) * 128],
                                                  rhs=D[:, fc % 2, cb * 512:(cb + 1) * 512], start=(fc == 0),
                                                  stop=(fc == 7)),
                         reads=['hidT', f'ring{ds_[fc // 2]}'], writes=[f'B{db}'], inc=(fc == 7))
                S.op('act', lambda e: e.activation(out=ybuf[:, st, cb * 512:(cb + 1) * 512], in_=B[db], func=AF.Copy),
                     reads=[f'B{db}'], writes=['ybuf', 'h2f'])
        for tt in range(8):
            for cb in range(4):
                sb2 = 7 if ctr['sc'] % 2 == 0 else 6
                ctr['sc'] += 1
                for st in range(2):
                    S.op('pe', lambda e: e.matmul(B[sb2], lhsT=PTe[:, st, tt * 128:(tt + 1) * 128],
                                                  rhs=ybuf[:, st, cb * 512:(cb + 1) * 512], start=(st == 0),
                                                  stop=(st == 1)),
                         reads=['PTe', 'ybuf'], writes=[f'B{sb2}'], inc=(st == 1))
                S.op('dve', lambda e: e.scalar_tensor_tensor(out=x1[:, tt, cb * 512:(cb + 1) * 512], in0=B[sb2],
                                                             scalar=comb[:, tt, ex:ex + 1],
                                                             in1=x1[:, tt, cb * 512:(cb + 1) * 512], op0=ALU.mult,
                                                             op1=ALU.add),
                     reads=[f'B{sb2}', 'comb', f'x1_{tt}'], writes=[f'x1_{tt}'])


    def run_pass(J):
        nonlocal nextload
        for ex in range(16):
            k = 12 * ex
            gs_ = [(k + 2 * q) % NR for q in range(4)]
            us_ = [(k + 2 * q + 1) % NR for q in range(4)]
            ds_ = [(k + 8 + q) % NR for q in range(4)]

            def after_q(q, k=k):
                nonlocal nextload
                while nextload < min(NU, k + 2 * (q + 1) + PRE):
                    load_unit(nextload)
                    nextload += 1

            emit_block(ex, J, (gs_, us_, ds_), after_q)
            while nextload < min(NU, k + 12 + PRE):
                load_unit(nextload)
                nextload += 1

    run_pass(0)

    S.barrier()
    S.dma('sp', gbc, g3_d, 'gbc', writes=['gbc'])
    S.op('dve', lambda e: e.memset(ssq, 0.0), reads=[f'ssqb{tt}' for tt in range(8)], writes=['ssq3'])
    outs = []
    ob = [h2f, A.f32(120 * KB, 2048)]
    for tt in range(8):
        oo = ob[tt % 2]
        okey = f'obuf{tt % 2}'
        extra = [f'ring{s}' for s in range(7)] + ['ybuf'] if tt < 2 else []
        S.op('act', lambda e: e.activation(out=oo, in_=x1[:, tt, :], func=AF.Square, accum_out=ssq[:, tt:tt + 1]),
             reads=[f'x1_{tt}', 'ssq3', 'h2f'], writes=[okey, f'ssqc{tt}'] + extra)
        S.op('act', lambda e: e.activation(out=rstd[:, tt:tt + 1], in_=ssq[:, tt:tt + 1], func=AF.Sqrt,
                                           scale=1.0 / 2048, bias=epst), reads=[f'ssqc{tt}', f'rstdb{tt}', 'epst'], writes=[f'rstdc{tt}'])
        S.op('dve', lambda e: e.reciprocal(out=rstd[:, tt:tt + 1], in_=rstd[:, tt:tt + 1]),
             reads=[f'rstdc{tt}'], writes=[f'rstdc{tt}'])
        S.op('dve', lambda e: e.scalar_tensor_tensor(out=oo, in0=x1[:, tt, :], scalar=rstd[:, tt:tt + 1], in1=gbc,
                                                     op0=ALU.mult, op1=ALU.mult),
             reads=[f'x1_{tt}', f'rstdc{tt}', 'gbc', okey], writes=[okey])
        outs.append(S.dma('sp', out_d[tt * 128:(tt + 1) * 128, :], oo, f'out{tt % 2}', reads=[okey]))
    return finish(outs)


_NC_CACHE = {}


def _prep_inputs(x, norm_mix, w_in, w_pool, pool_scale, w_branch_pool, w_branch_attn, w_out, norm_ffn, w_r_group,
                 b_r_group, w_r_expert, b_r_expert, w_gate, w_up, w_down, norm_final):
    f = np.float32
    x2 = np.asarray(x, f)[0]
    bc = lambda v: np.ascontiguousarray(np.broadcast_to(np.asarray(v, f).reshape(1, -1), (128, np.asarray(v).size)))
    wr = np.concatenate([np.asarray(w_r_group, f)[0]] + [np.asarray(w_r_expert, f)[0][g] for g in range(4)], axis=1)
    wr = np.ascontiguousarray(wr.reshape(16, 128, 20).transpose(1, 0, 2).reshape(128, 320))
    brv = np.concatenate([np.asarray(b_r_group, f)[0], np.asarray(b_r_expert, f)[0].reshape(16)])
    common = {
        "w_in": np.ascontiguousarray(np.asarray(w_in, f)[0]),
        "w_pool": np.ascontiguousarray(np.asarray(w_pool, f)[0]),
        "w_bp": np.ascontiguousarray(np.asarray(w_branch_pool, f)[0]),
        "w_ba": np.ascontiguousarray(np.asarray(w_branch_attn, f)[0]),
        "w_out": np.ascontiguousarray(np.asarray(w_out, f)[0]),
        "w_gate": np.ascontiguousarray(np.asarray(w_gate, f)[0]),
        "w_up": np.ascontiguousarray(np.asarray(w_up, f)[0]),
        "w_down": np.ascontiguousarray(np.asarray(w_down, f)[0]),
        "g1": bc(np.asarray(norm_mix)[0]), "g2": bc(np.asarray(norm_ffn)[0]), "g3": bc(norm_final),
        "pst": np.ascontiguousarray(np.asarray(pool_scale, f)[0].reshape(8, 128).T),
        "wr": wr, "br": bc(brv),
    }
    in_maps = []
    for c in range(NCORES):
        n = 1024 * (c + 1)
        xcx = np.zeros((8192, 2048), f)
        xcx[8192 - n:] = x2[:n]
        pm = np.zeros((128, 32), f)
        pm[:, :28 - 4 * c] = NEG
        ic = np.zeros((128, 8, 16), f)
        for ch in range(8):
            w = 2 ** (ch // 2 + 1)
            for t in range(16):
                ic[:, ch, t] = 1.0 / min(1024 * c + t + 1, w)
        m = dict(common)
        m["xc"] = xcx
        m["padmask"] = pm
        m["invcnt"] = ic.reshape(128, 128)
        in_maps.append(m)
    return in_maps


def kernel(**inputs):
    in_maps = _prep_inputs(**inputs)
    if "nc" not in _NC_CACHE:
        _NC_CACHE["nc"] = build()
    nc = _NC_CACHE["nc"]
    res = run_bass_kernel_spmd(nc, in_maps, core_ids=list(range(NCORES)))
    out = np.concatenate([np.asarray(r["out"], np.float32) for r in res.results], axis=0)
    return out.reshape(1, 8192, 2048)
```
